# Optimizing a Trainium2 kernel written in Bass

```python
import jax, jax.numpy as jnp
from jax import lax
import numpy as np


D_MODEL = 1024
BATCH = 16
SEQ = 2048
DEPTH = 2

D_MIX = D_MODEL
HG_WIDTH = D_MIX // 2
HG_HEADS = 4
HG_DK = HG_WIDTH // HG_HEADS
HG_DV = HG_WIDTH // HG_HEADS
HG_CHUNK = 16
AT_WIDTH = D_MIX - HG_WIDTH
AT_HEADS = 8
AT_DH = AT_WIDTH // AT_HEADS
MOBA_BLOCK = 256
MOBA_TOPK = 3
MOBA_QCHUNK = 16
IN_SPLITS = (HG_WIDTH, HG_WIDTH, HG_WIDTH, HG_WIDTH, AT_WIDTH, AT_WIDTH, AT_WIDTH, AT_WIDTH)
IN_COLS = sum(IN_SPLITS)
NORM_EPS = 1e-6
MASK_VALUE = -1e30
LB_MAX = 1.0 - 1e-6

kernel_name = "hymba_style_hgrn2_moba_block"


def _rms_norm(x, w):
    xf = x.astype(jnp.float32)
    y = xf * lax.rsqrt(jnp.mean(xf * xf, axis=-1, keepdims=True) + NORM_EPS)
    return (y * w.astype(jnp.float32)).astype(x.dtype)


def _alibi_slopes(n_heads):
    return jnp.asarray([2.0 ** (-8.0 * (h + 1) / n_heads) for h in range(n_heads)], dtype=jnp.float32)


def _hgrn_lower_bounds(lb_logits):
    p = jax.nn.softmax(lb_logits.astype(jnp.float32), axis=0)
    return jnp.clip(jnp.cumsum(p, axis=0) - p[0:1], 0.0, LB_MAX)


def _hgrn2(q, f_logit, i, lb):
    B, T, _ = q.shape
    C = HG_CHUNK
    N = T // C

    def heads(z):
        return z.astype(jnp.float32).reshape(B, N, C, HG_HEADS, -1).transpose(0, 3, 1, 2, 4)

    q = jax.nn.silu(heads(q))
    lbh = lb.astype(jnp.float32).reshape(HG_HEADS, 1, 1, HG_DK)
    f = lbh + (1.0 - lbh) * jax.nn.sigmoid(heads(f_logit))
    log_f = jnp.logaddexp(jnp.log(lbh), jnp.log1p(-lbh) + jax.nn.log_sigmoid(heads(f_logit)))
    k = 1.0 - f
    i = heads(i)
    b = jnp.cumsum(log_f, axis=3)
    b_last = b[:, :, :, C - 1:]
    causal = jnp.tril(jnp.ones((C, C), dtype=bool))[:, :, None]
    pair = b[:, :, :, :, None, :] - b[:, :, :, None, :, :]
    decay_ts = jnp.exp(jnp.where(causal, pair, -jnp.inf))
    scores = jnp.einsum("bhncd,bhnsd,bhncsd->bhncs", q, k, decay_ts)
    o_intra = jnp.einsum("bhncs,bhnse->bhnce", scores, i)
    q_in = q * jnp.exp(b)
    k_st = k * jnp.exp(b_last - b)
    decay = jnp.exp(b_last[:, :, :, 0])

    def step(S, xs):
        qn, kn, in_, dn = xs
        o = jnp.einsum("bhcd,bhde->bhce", qn, S)
        S = dn[..., None] * S + jnp.einsum("bhcd,bhce->bhde", kn, in_)
        return S, o

    S0 = jnp.zeros((B, HG_HEADS, HG_DK, HG_DV), jnp.float32)
    mv = lambda z: jnp.moveaxis(z, 2, 0)
    _, o_inter = lax.scan(step, S0, (mv(q_in), mv(k_st), mv(i), mv(decay)))
    o = o_intra + jnp.moveaxis(o_inter, 0, 2)
    return o.transpose(0, 2, 3, 1, 4).reshape(B, T, HG_HEADS, HG_DV)


def _moba(q, k, v, q_gain, k_gain):
    B, T, _ = q.shape
    heads = lambda z: z.reshape(B, T, AT_HEADS, AT_DH).transpose(0, 2, 1, 3)
    q = _rms_norm(heads(q), q_gain).astype(jnp.float32)
    k = _rms_norm(heads(k), k_gain).astype(jnp.float32)
    v = heads(v).astype(jnp.float32)

    nb = -(-T // MOBA_BLOCK)
    t_pad = nb * MOBA_BLOCK
    pad = ((0, 0), (0, 0), (0, t_pad - T), (0, 0))
    k_pad = jnp.pad(k, pad)
    v_pad = jnp.pad(v, pad)
    kb = k_pad.reshape(B, AT_HEADS, nb, MOBA_BLOCK, AT_DH)
    vb = v_pad.reshape(B, AT_HEADS, nb, MOBA_BLOCK, AT_DH)

    k_mean = jnp.mean(kb, axis=3)
    cur_blk = jnp.arange(T) // MOBA_BLOCK
    past = jnp.arange(nb)[None, :] < cur_blk[:, None]
    gate = jnp.where(past, jnp.einsum("bhtd,bhnd->bhtn", q, k_mean), MASK_VALUE)
    n_sel = min(MOBA_TOPK, nb)
    _, sel = lax.top_k(gate, n_sel)
    valid = sel < cur_blk[:, None]

    nq = T // MOBA_QCHUNK
    chunk = lambda z: jnp.moveaxis(z.reshape(B, AT_HEADS, nq, MOBA_QCHUNK, *z.shape[3:]), 2, 0)
    t0s = jnp.arange(nq, dtype=jnp.int32) * MOBA_QCHUNK
    slopes = _alibi_slopes(AT_HEADS)[None, :, None, None]
    bi = jnp.arange(B)[:, None, None, None]
    hi = jnp.arange(AT_HEADS)[None, :, None, None]
    offs = jnp.arange(MOBA_BLOCK)
    scale = AT_DH ** -0.5

    def attend(xs):
        qc, selc, validc, t0 = xs
        t = t0 + jnp.arange(MOBA_QCHUNK)
        blk0 = (t0 // MOBA_BLOCK) * MOBA_BLOCK
        k_own = lax.dynamic_slice_in_dim(k_pad, blk0, MOBA_BLOCK, axis=2)
        v_own = lax.dynamic_slice_in_dim(v_pad, blk0, MOBA_BLOCK, axis=2)
        dist_own = (t[:, None] - (blk0 + offs)[None, :]).astype(jnp.float32)
        s_own = jnp.einsum("bhqd,bhsd->bhqs", qc, k_own) * scale - slopes * dist_own
        s_own = jnp.where(dist_own >= 0, s_own, MASK_VALUE)
        k_sel = kb[bi, hi, selc]
        v_sel = vb[bi, hi, selc]
        dist_sel = (t[:, None, None] - (selc[..., None] * MOBA_BLOCK + offs)).astype(jnp.float32)
        s_sel = jnp.einsum("bhqd,bhqjsd->bhqjs", qc, k_sel) * scale - slopes[..., None] * dist_sel
        s_sel = jnp.where(validc[..., None], s_sel, MASK_VALUE)
        s_all = jnp.concatenate(
            [s_own, s_sel.reshape(B, AT_HEADS, MOBA_QCHUNK, n_sel * MOBA_BLOCK)], axis=-1)
        p = jax.nn.softmax(s_all, axis=-1)
        p_own = p[..., :MOBA_BLOCK]
        p_sel = p[..., MOBA_BLOCK:].reshape(B, AT_HEADS, MOBA_QCHUNK, n_sel, MOBA_BLOCK)
        return (jnp.einsum("bhqs,bhsd->bhqd", p_own, v_own)
                + jnp.einsum("bhqjs,bhqjsd->bhqd", p_sel, v_sel))

    out = lax.map(attend, (chunk(q), chunk(sel), chunk(valid), t0s))
    out = jnp.moveaxis(out, 0, 2).reshape(B, AT_HEADS, T, AT_DH)
    return out.transpose(0, 2, 1, 3).reshape(B, T, AT_WIDTH)


def setup_inputs(seed: int = 0) -> dict:
    key = jax.random.key(seed)
    ks = jax.random.split(key, 8)
    x = jax.random.normal(ks[0], (BATCH, SEQ, D_MODEL), jnp.float32)
    norm_w = 1.0 + 0.02 * jax.random.normal(ks[1], (DEPTH, D_MODEL), jnp.float32)
    w_in = jax.random.normal(ks[2], (DEPTH, D_MODEL, IN_COLS), jnp.float32) * D_MODEL ** -0.5
    hg_lb_logits = 0.5 * jax.random.normal(ks[3], (DEPTH, HG_WIDTH), jnp.float32)
    hg_norm_w = 1.0 + 0.02 * jax.random.normal(ks[4], (DEPTH, HG_DV), jnp.float32)
    q_norm_w = 1.0 + 0.02 * jax.random.normal(ks[5], (DEPTH, AT_DH), jnp.float32)
    k_norm_w = 1.0 + 0.02 * jax.random.normal(ks[6], (DEPTH, AT_DH), jnp.float32)
    w_out = jax.random.normal(ks[7], (DEPTH, D_MIX, D_MODEL), jnp.float32) * D_MIX ** -0.5
    return {"x": x, "norm_w": norm_w, "w_in": w_in, "hg_lb_logits": hg_lb_logits,
            "hg_norm_w": hg_norm_w, "q_norm_w": q_norm_w, "k_norm_w": k_norm_w, "w_out": w_out}


def reference(x, norm_w, w_in, hg_lb_logits, hg_norm_w, q_norm_w, k_norm_w, w_out):
    B, T, _ = x.shape
    lower_bounds = _hgrn_lower_bounds(hg_lb_logits)
    split_points = [int(s) for s in np.cumsum(IN_SPLITS)[:-1]]
    for l in range(DEPTH):
        h = _rms_norm(x, norm_w[l])
        proj = jnp.einsum("btd,dc->btc", h, w_in[l])
        hq, hf, hi, hg, aq, ak, av, ag = jnp.split(proj, split_points, axis=-1)
        o_h = _rms_norm(_hgrn2(hq, hf, hi, lower_bounds[l]), hg_norm_w[l]).reshape(B, T, HG_WIDTH)
        o_h = o_h * jax.nn.silu(hg.astype(jnp.float32))
        o_a = _moba(aq, ak, av, q_norm_w[l], k_norm_w[l]) * jax.nn.silu(ag.astype(jnp.float32))
        mixed = jnp.concatenate([o_h, o_a], axis=-1).astype(x.dtype)
        x = x + jnp.einsum("btc,cd->btd", mixed, w_out[l])
    return x
```

```python
import numpy as np
import ml_dtypes
from contextlib import ExitStack
import concourse.bass as bass
import concourse.mybir as mybir
from concourse.bass_utils import run_bass_kernel_spmd

F32 = mybir.dt.float32
BF16 = mybir.dt.bfloat16
U32 = mybir.dt.uint8
ALU = mybir.AluOpType
AF = mybir.ActivationFunctionType
AX = mybir.AxisListType

NT = 16
NSEQ = 2
EPS = 1e-6
NEG = -30000.0


class Sched:
    REAL = ("pe", "act", "dve", "pool", "sp")

    def __init__(self, nc, n_dma_sems=8):
        self.nc = nc
        self.ops = {e: [] for e in self.REAL}
        self.nops = {}
        self.clock = {e: {} for e in self.REAL}
        self.opclock = {}
        self.lastw = {}
        self.readers = {}
        self.signaled = set()
        self.n_dma_sems = n_dma_sems
        self.dma_rr = {"sp": 0, "pool": 0, "act": 0}
        self.epochs = {e: [0] for e in self.REAL}
        self.strict = True

    def new_epoch(self):
        for e in self.REAL:
            self.epochs[e].append(len(self.ops[e]))

    def _deps(self, eng, r, w):
        raw, other = set(), set()
        for k in r:
            if k in self.lastw:
                raw.add(self.lastw[k])
        for k in w:
            if k in self.lastw:
                other.add(self.lastw[k])
            for d in self.readers.get(k, ()):
                other.add(d)
        deps = set(raw)
        for d in other:
            if d[0] != eng or (self.strict and eng != "pe"):
                deps.add(d)
        return deps

    def _apply_waits(self, eng, deps):
        clk = self.clock[eng]
        waits = []
        best = {}
        for (e2, i2) in deps:
            if best.get(e2, -1) < i2:
                best[e2] = i2
        for e2, i2 in best.items():
            if clk.get(e2, -1) >= i2:
                continue
            waits.append((e2, i2))
            self.signaled.add((e2, i2))
            oc = self.opclock.get((e2, i2), {})
            for k, v in oc.items():
                if clk.get(k, -1) < v:
                    clk[k] = v
            if clk.get(e2, -1) < i2:
                clk[e2] = i2
        return waits

    def _record(self, me, r, w):
        for k in r:
            self.readers.setdefault(k, set()).add(me)
        for k in w:
            self.lastw[k] = me
            self.readers[k] = set()

    def op(self, eng, fn, r=(), w=()):
        deps = self._deps(eng, r, w)
        waits = self._apply_waits(eng, deps)
        idx = len(self.ops[eng])
        self.ops[eng].append(dict(fn=fn, waits=waits, dma=None))
        snap = dict(self.clock[eng])
        snap[eng] = idx
        self.opclock[(eng, idx)] = snap
        me = (eng, idx)
        self._record(me, r, w)
        return me

    def dma(self, q, fn, r=(), w=()):
        slot = self.dma_rr[q]
        self.dma_rr[q] = (slot + 1) % (4 if q == "pool" else self.n_dma_sems)
        pe = "dma_%s_%d" % (q, slot)
        pidx = self.nops.get(pe, 0)
        self.nops[pe] = pidx + 1
        deps = self._deps(pe, r, w)
        if pidx > 0:
            deps.add((pe, pidx - 1))
        waits = self._apply_waits(q, deps)
        self.ops[q].append(dict(fn=fn, waits=waits, dma=(pe, pidx)))
        snap = dict(self.clock[q])
        snap[pe] = pidx
        self.opclock[(pe, pidx)] = snap
        me = (pe, pidx)
        self._record(me, r, w)
        return me

    def finish_wait(self, eng, deps):
        waits = self._apply_waits(eng, set(deps))
        self.ops[eng].append(dict(fn=None, waits=waits, dma=None))

    def emit(self, stack):
        nc = self.nc
        sems = {}
        val = {}
        for e in self.REAL:
            bounds = self.epochs[e] + [len(self.ops[e]) + 1]
            ep = 0
            c = 0
            cur = None
            for i in range(len(self.ops[e])):
                while i >= bounds[ep + 1]:
                    ep += 1
                    c = 0
                    cur = None
                if (e, i) in self.signaled:
                    if cur is None:
                        cur = stack.enter_context(nc.semaphore("s_%s_%d" % (e, ep)))
                    c += 1
                    val[(e, i)] = (cur, c)
        for pe, n in self.nops.items():
            sm = stack.enter_context(nc.semaphore("s_" + pe))
            sems[pe] = sm
            for i in range(n):
                val[(pe, i)] = (sm, 16 * (i + 1))
        block = stack.enter_context(nc.Block())
        binders = {"pe": block.tensor, "act": block.scalar, "dve": block.vector,
                   "pool": block.gpsimd, "sp": block.sync}

        def mk(e):
            ops = self.ops[e]

            def body(engine):
                for i, o in enumerate(ops):
                    for d in o["waits"]:
                        sm, v = val[d]
                        engine.wait_ge(sm, v)
                    if o["fn"] is None:
                        continue
                    ins = o["fn"](engine)
                    if o["dma"] is not None:
                        ins.then_inc(sems[o["dma"][0]], 16)
                    elif (e, i) in self.signaled:
                        ins.then_inc(val[(e, i)][0], 1)
            return body

        for e in self.REAL:
            if self.ops[e]:
                binders[e](mk(e))


def _consts():
    c = {}
    c["ident"] = np.eye(128, dtype=np.float32).astype(ml_dtypes.bfloat16)
    c["onesb"] = np.ones((128, 128), np.float32).astype(ml_dtypes.bfloat16)
    rm = np.ones((128, 512), np.float32)
    rm[:, ::32] = 0
    c["rmask"] = rm.astype(ml_dtypes.bfloat16)
    s = np.arange(128)[:, None]
    t = np.arange(128)[None, :]
    am = ((s // 32 == t // 32) & (t >= s)).astype(np.uint8)
    c["amask"] = np.ascontiguousarray(np.tile(am, (1, 4)))
    c["cmask"] = np.where(s > t, NEG, 0.0).astype(np.float32).astype(ml_dtypes.bfloat16)
    lk = np.zeros((128, NT, 128), np.float32)
    for g in range(4):
        for kt in range(NT):
            lk[32 * g + kt // 2, kt, :] = 1.0
            lk[32 * g + 8, kt, :] = np.arange(128)
            lk[32 * g + 9, kt, :] = 1.0
            lk[32 * g + 10, kt, :] = 1.0
            lk[32 * g + 11, kt, :] = 128.0 * kt
    c["lk"] = lk.astype(ml_dtypes.bfloat16)
    c["cmask2"] = np.ascontiguousarray(np.concatenate([c["cmask"], c["cmask"]], axis=1))
    gm = np.zeros((128, 4), np.float32)
    for g in range(4):
        gm[32 * g:32 * g + 32, g] = 1.0
    c["gmask"] = gm
    ct = np.zeros((128, NT, 8, 4), np.float32)
    for h in range(8):
        sl = 2.0 ** (-(h + 1))
        for qt in range(NT):
            ct[:, qt, h, 0] = sl
            ct[:, qt, h, 1] = -sl * np.arange(128)
            ct[:, qt, h, 2] = -sl * 128.0 * qt
            ct[:, qt, h, 3] = sl
    c["ctab"] = ct.astype(ml_dtypes.bfloat16)
    return c


def _build(layers, first_layer_of_module, nseq=NSEQ, ntiles=NT, dbg=None):
    nc = bass.Bass("TRN2", target_bir_lowering=False, dynamic_dma_scratch_size=2048)
    D = nc.dram_tensor
    x_d = D("x", [NSEQ, 2048, 1024], F32, kind="ExternalInput").ap()
    y_d = D("y", [NSEQ, 2048, 1024], F32, kind="ExternalOutput").ap()
    win_d = D("w_in", [2, 1024, 4096], F32, kind="ExternalInput").ap()
    wout_d = D("w_out", [2, 1024, 1024], F32, kind="ExternalInput").ap()
    nw_d = D("norm_w", [2, 1024], F32, kind="ExternalInput").ap()
    lb_d = D("hg_lb_logits", [2, 512], F32, kind="ExternalInput").ap()
    hgw_d = D("hg_norm_w", [2, 128], F32, kind="ExternalInput").ap()
    qw_d = D("q_norm_w", [2, 64], F32, kind="ExternalInput").ap()
    kw_d = D("k_norm_w", [2, 64], F32, kind="ExternalInput").ap()
    ident_d = D("ident", [128, 128], BF16, kind="ExternalInput").ap()
    ones_d = D("onesb", [128, 128], BF16, kind="ExternalInput").ap()
    rmask_d = D("rmask", [128, 512], BF16, kind="ExternalInput").ap()
    amask_d = D("amask", [128, 512], U32, kind="ExternalInput").ap()
    cmask_d = D("cmask", [128, 128], BF16, kind="ExternalInput").ap()
    lk_d = D("lk", [128, NT, 128], BF16, kind="ExternalInput").ap()
    ctab_d = D("ctab", [128, NT, 8, 4], BF16, kind="ExternalInput").ap()
    cmask2_d = D("cmask2", [128, 256], BF16, kind="ExternalInput").ap()
    gmask_d = D("gmask", [128, 4], F32, kind="ExternalInput").ap()
    dbg_d = {}
    if dbg is not None:
        for nm, shp, dt in [("d_hT", [128, 1024], BF16), ("d_qe", [128, 512], BF16), ("d_ke", [128, 512], BF16),
                            ("d_A", [128, 512], BF16), ("d_S", [128, 512], BF16), ("d_mixT", [128, 1024], BF16),
                            ("d_qT", [128, 512], BF16), ("d_g", [128, 64], F32), ("d_selbp", [128, 256], BF16),
                            ("d_bl", [128, 512], F32), ("d_i", [128, 512], BF16), ("d_kt", [128, 512], BF16)]:
            dbg_d[nm] = D(nm, shp, dt, kind="ExternalOutput").ap()

    with ExitStack() as st:
        T = lambda name, shape, dt: st.enter_context(nc.sbuf_tensor(name, shape, dt))
        x_sb = T("x_sb", [128, NT, 1024], F32)
        w_sb = T("w_sb", [128, 8, 4096], BF16)
        wo_sb = T("wo_sb", [128, 8, 1024], BF16)
        KT_sb = T("KT_sb", [128, 4, 2048], BF16)
        V_sb = T("V_sb", [128, NT, 512], BF16)
        h_bf = T("h_bf", [128, 1024], BF16)
        hT = T("hT", [128, 8, 128], BF16)
        qf = T("qf", [128, 512], BF16)
        fb = T("fb", [128, 512], F32)
        th = T("th", [128, 512], F32)
        bl = T("bl", [128, 512], F32)
        eb = T("eb", [128, 512], BF16)
        qe = T("qe", [128, 512], BF16)
        ke = T("ke", [128, 512], BF16)
        keT = T("keT", [128, 512], BF16)
        A_sb = T("A_sb", [128, 512], BF16)
        S_sb = T("S_sb", [128, 4, 128], BF16)
        i_sb = [T("i_sb0", [128, 512], BF16), T("i_sb1", [128, 512], BF16)]
        sg = [T("sg0", [128, 512], BF16), T("sg1", [128, 512], BF16)]
        sga = [T("sga0", [128, 512], BF16), T("sga1", [128, 512], BF16)]
        mixT = T("mixT", [128, 8, 128], BF16)
        qkn = T("qkn", [128, 1024], BF16)
        qTz = T("qTz", [128, 8, 128], BF16)
        vst = T("vst", [128, 512], BF16)
        rdn = T("rdn", [128, 2, 128], F32)
        oa = T("oa", [128, 2, 128], F32)
        cmp = oa[:].rearrange("p a t -> p (a t)").bitcast(BF16)
        g_sb = T("g_sb", [128, 64], F32)
        rank = T("rank", [128, 64], F32)
        selbp = T("selbp", [128, 8, 32], BF16)
        selbTz = T("selbTz", [128, 8, 128], BF16)
        cmask2 = T("cmask2_sb", [128, 256], BF16)
        gmask = T("gmask_sb", [128, 4], F32)
        PT = [T("PT0", [128, 512], BF16), T("PT1", [128, 512], BF16)]
        ident = T("ident_sb", [128, 128], BF16)
        onesb = T("ones_sb", [128, 128], BF16)
        rmask = T("rmask_sb", [128, 512], BF16)
        amask = T("amask_sb", [128, 512], U32)
        cmask = T("cmask_sb", [128, 128], BF16)
        Lk = T("lk_sb", [128, NT, 128], BF16)
        ctab = T("ctab_sb", [128, NT, 8, 4], BF16)
        KM = T("KM", [128, 4, 16], BF16)
        kms = T("kms", [128, 4], F32)
        nwT = T("nwT", [128, 2, 8], F32)
        lbl = T("lbl", [128, 2, 4], F32)
        cst = T("cst", [128, 2, 3, 4], F32)
        hgw = T("hgw", [128, 2], F32)
        qg2 = T("qg2", [128, 2], F32)
        kg2 = T("kg2", [128, 2], F32)
        ss = T("ss", [128, 4], F32)
        ss2 = T("ss2", [128, 16], F32)
        rs2 = T("rs2", [128, 16], F32)
        tmp4 = T("tmp4", [128, 4], F32)
        B = [st.enter_context(nc.psum_tensor("B%d" % i, [128, 512], F32)) for i in range(8)]
        Bb = [b[:].bitcast(BF16) for b in B]

        s = Sched(nc)
        op, dma = s.op, s.dma

        for sbt, drt, key in [(ident, ident_d, "ident"), (onesb, ones_d, "onesb"), (rmask, rmask_d, "rmask"),
                              (amask, amask_d, "amask"), (cmask, cmask_d, "cmask"), (Lk, lk_d, "Lk"), (ctab, ctab_d, "ctab"),
                              (cmask2, cmask2_d, "cmask2"), (gmask, gmask_d, "gmask")]:
            dma("sp", lambda e, a=sbt, b=drt: e.dma_start(out=a[:], in_=b[:]), w=[key])
        with nc.allow_non_contiguous_dma(reason="tiny param loads"):
            dma("sp", lambda e: e.dma_start(out=nwT[:], in_=nw_d.rearrange("l (k p) -> p l k", p=128), allow_slow_non_contiguous=True), w=["nwT"])
            dma("sp", lambda e: e.dma_start(out=lbl[:], in_=lb_d.rearrange("l (h p) -> p l h", p=128), allow_slow_non_contiguous=True), w=["lbl"])
            dma("sp", lambda e: e.dma_start(out=hgw[:], in_=hgw_d.rearrange("l p -> p l"), allow_slow_non_contiguous=True), w=["hgw"])
            dma("sp", lambda e: e.dma_start(out=qg2[0:64, :], in_=qw_d.rearrange("l p -> p l"), allow_slow_non_contiguous=True), w=["qg2a"])
            dma("sp", lambda e: e.dma_start(out=qg2[64:128, :], in_=qw_d.rearrange("l p -> p l"), allow_slow_non_contiguous=True), w=["qg2b"])
            dma("sp", lambda e: e.dma_start(out=kg2[0:64, :], in_=kw_d.rearrange("l p -> p l"), allow_slow_non_contiguous=True), w=["kg2a"])
            dma("sp", lambda e: e.dma_start(out=kg2[64:128, :], in_=kw_d.rearrange("l p -> p l"), allow_slow_non_contiguous=True), w=["kg2b"])
        op("dve", lambda e: e.tensor_scalar(out=qg2[:], in0=qg2[:], scalar1=0.125, scalar2=None, op0=ALU.mult),
           r=["qg2a", "qg2b"], w=["qg2"])
        op("dve", lambda e: e.tensor_tensor(out=tmp4[:], in0=lbl[:, 0, :], in1=lbl[:, 1, :], op=ALU.subtract), r=["lbl"], w=["tmp4"])
        op("act", lambda e: e.activation(out=tmp4[:], in_=tmp4[:], func=AF.Exp), r=["tmp4"], w=["tmp4"])
        op("dve", lambda e: e.tensor_scalar(out=tmp4[:], in0=tmp4[:], scalar1=1.0, scalar2=None, op0=ALU.add), r=["tmp4"], w=["tmp4"])
        op("dve", lambda e: e.reciprocal(out=tmp4[:], in_=tmp4[:]), r=["tmp4"], w=["tmp4"])
        op("dve", lambda e: e.tensor_scalar(out=tmp4[:], in0=tmp4[:], scalar1=1.0 - 1e-6, scalar2=0.0, op0=ALU.min, op1=ALU.max), r=["tmp4"], w=["tmp4"])
        op("dve", lambda e: e.memset(cst[:, 0, 0, :], 0.5), w=["cst0a"])
        op("dve", lambda e: e.memset(cst[:, 0, 1, :], 0.5), w=["cst0b"])
        op("dve", lambda e: e.memset(cst[:, 0, 2, :], -0.5), w=["cst0c"])
        op("dve", lambda e: e.tensor_scalar(out=cst[:, 1, 0, :], in0=tmp4[:], scalar1=-0.5, scalar2=0.5, op0=ALU.mult, op1=ALU.add), r=["tmp4"], w=["cst1a"])
        op("dve", lambda e: e.tensor_scalar(out=cst[:, 1, 1, :], in0=tmp4[:], scalar1=0.5, scalar2=0.5, op0=ALU.mult, op1=ALU.add), r=["tmp4"], w=["cst1b"])
        op("dve", lambda e: e.tensor_scalar(out=cst[:, 1, 2, :], in0=tmp4[:], scalar1=0.5, scalar2=-0.5, op0=ALU.mult, op1=ALU.add), r=["tmp4"], w=["cst1c"])
        CST = ["cst0a", "cst0b", "cst0c", "cst1a", "cst1b", "cst1c"]
        op("pool", lambda e: e.memset(A_sb[:], 0.0), w=["A_sb"])
        op("pool", lambda e: e.memset(selbp[:], 0.0), w=["selbp"])
        op("pool", lambda e: e.memset(KM[:], 0.0), w=["KM"])
        op("pool", lambda e: e.memset(qTz[:], 0.0), w=["qTz0"])

        def load_w_in(l):
            for k in range(8):
                for hf in range(2):
                    dma("pool", lambda e, k=k, hf=hf, l=l: e.dma_start(out=w_sb[:, k, hf * 2048:(hf + 1) * 2048],
                                                                       in_=win_d[l, k * 128:(k + 1) * 128, hf * 2048:(hf + 1) * 2048]),
                        w=["win_%d_%d" % (k, hf)])

        def load_w_out(l):
            for cc in range(8):
                dma("pool", lambda e, cc=cc, l=l: e.dma_start(out=wo_sb[:, cc, :], in_=wout_d[l, cc * 128:(cc + 1) * 128, :]),
                    w=["wo_%d" % cc])

        passes = [(sq, l) for sq in range(nseq) for l in layers]
        load_w_in(passes[0][1])
        load_w_out(passes[0][1])
        out_dmas = []
        FBK = ["fb_%d" % h for h in range(4)]
        KKK = ["kk_%d" % h for h in range(4)]
        THK = ["th_%d" % h for h in range(4)]

        def dump(name, ap, keys):
            out_dmas.append(dma("sp", lambda e, a=ap, n=name: e.dma_start(out=dbg_d[n][:], in_=a), r=keys, w=["dbgout_" + name]))

        jobs = []
        for pi, (sq, l) in enumerate(passes):
            for t in range(ntiles):
                jobs.append(dict(n=len(jobs), pi=pi, sq=sq, l=l, t=t))

        def xload(sq, t):
            dma("sp", lambda e, t=t, sq=sq: e.dma_start(out=x_sb[:, t, :], in_=x_d[sq, t * 128:(t + 1) * 128, :]), w=["x_%d" % t])

        def stageA(job):
            n, pi, sq, l, t = job["n"], job["pi"], job["sq"], job["l"], job["t"]
            nb = n % 2
            D_ = (dbg is not None and dbg == (sq, l, t))
            xk = "x_%d" % t
            nxt = passes[pi + 1] if pi + 1 < len(passes) else None
            if l == layers[0]:
                if t == 0:
                    xload(sq, 0)
                if t + 1 < ntiles:
                    xload(sq, t + 1)
            op("act", lambda e, t=t: e.activation(out=h_bf[:], in_=x_sb[:, t, :], func=AF.Square, accum_out=ss[:, 0:1]), r=[xk], w=["h_bf", "ss"])
            op("act", lambda e: e.activation(out=ss[:, 1:2], in_=ss[:, 0:1], func=AF.Ln, scale=1.0 / 1024, bias=EPS), r=["ss"], w=["ss1"])
            op("act", lambda e: e.activation(out=ss[:, 2:3], in_=ss[:, 1:2], func=AF.Exp, scale=-0.5), r=["ss1"], w=["ss2"])
            op("dve", lambda e, t=t: e.tensor_scalar(out=h_bf[:], in0=x_sb[:, t, :], scalar1=ss[:, 2:3], scalar2=None, op0=ALU.mult), r=[xk, "ss2"], w=["h_bf"])
            for k in range(8):
                op("pe", lambda e, k=k: e.transpose(Bb[0][:, k * 128:(k + 1) * 128], h_bf[:, k * 128:(k + 1) * 128], ident[:]), r=["h_bf", "ident"], w=["B0"])
            op("dve", lambda e, l=l: e.tensor_tensor(out=hT[:], in0=Bb[0].rearrange("p (k t) -> p k t", k=8),
                                                    in1=nwT[:, l, :].unsqueeze(2).to_broadcast([128, 8, 128]), op=ALU.mult), r=["B0", "nwT"], w=["hT"])
            if D_:
                dump("d_hT", hT[:].rearrange("p k t -> p (k t)"), ["hT"])
            yield

            def fm(c0, bi):
                for j in range(4):
                    for k in range(8):
                        cc0 = c0 + j * 128
                        op("pe", lambda e, j=j, k=k, cc0=cc0, bi=bi: e.matmul(B[bi][:, j * 128:(j + 1) * 128], lhsT=w_sb[:, k, cc0:cc0 + 128], rhs=hT[:, k, :],
                                                                            start=(k == 0), stop=(k == 7)),
                           r=["hT", "win_%d_%d" % (k, cc0 // 2048)], w=["B%d" % bi])

            def tm(c0, bi):
                for k in range(8):
                    op("pe", lambda e, k=k, c0=c0, bi=bi: e.matmul(B[bi][:, 0:512], lhsT=hT[:, k, :], rhs=w_sb[:, k, c0:c0 + 512], start=(k == 0), stop=(k == 7)),
                       r=["hT", "win_%d_%d" % (k, c0 // 2048)], w=["B%d" % bi])

            def ev_v(bi):
                if t == 0 and n > 0:
                    op("act", lambda e, bi=bi: e.copy(out=vst[:], in_=B[bi][:]), r=["B%d" % bi], w=["vst"])
                else:
                    op("act", lambda e, t=t, bi=bi: e.copy(out=V_sb[:, t, :], in_=B[bi][:]), r=["B%d" % bi], w=["V_%d" % t])
            glist = [
                (lambda bi: tm(3072, bi), ev_v),
                (lambda bi: tm(1024, bi), lambda bi: op("dve", lambda e, nb=nb, bi=bi: e.tensor_copy(out=i_sb[nb][:], in_=B[bi][:]), r=["B%d" % bi], w=["i_sb%d" % nb])),
                (lambda bi: tm(2048, bi), lambda bi: op("act", lambda e, bi=bi: e.copy(out=qkn[:, 0:512], in_=B[bi][:]), r=["B%d" % bi], w=["qkn_a"])),
                (lambda bi: tm(2560, bi), lambda bi: op("act", lambda e, bi=bi: e.copy(out=qkn[:, 512:1024], in_=B[bi][:]), r=["B%d" % bi], w=["qkn_b"])),
                (lambda bi: fm(1536, bi), lambda bi: op("act", lambda e, nb=nb, bi=bi: e.copy(out=sg[nb][:], in_=B[bi][:]), r=["B%d" % bi], w=["sg%d" % nb])),
                (lambda bi: fm(3584, bi), lambda bi: op("act", lambda e, nb=nb, bi=bi: e.copy(out=sga[nb][:], in_=B[bi][:]), r=["B%d" % bi], w=["sga%d" % nb])),
                (lambda bi: fm(512, bi), lambda bi: op("act", lambda e, bi=bi: e.copy(out=th[:], in_=B[bi][:]), r=["B%d" % bi], w=THK)),
                (lambda bi: fm(0, bi), lambda bi: op("act", lambda e, bi=bi: e.copy(out=qf[:], in_=B[bi][:]), r=["B%d" % bi], w=["qf"])),
            ]
            AB = [3, 7]
            for gk, (mmf, evf) in enumerate(glist):
                mmf(AB[gk % 2])
                if gk > 0:
                    glist[gk - 1][1](AB[(gk - 1) % 2])
                yield
            if t == ntiles - 1 and nxt is not None and nxt[1] != l:
                load_w_in(nxt[1])
            glist[-1][1](AB[(len(glist) - 1) % 2])
            yield
            op("act", lambda e: e.activation(out=qf[:], in_=qf[:], func=AF.Silu), r=["qf"], w=["qf"])
            op("act", lambda e: e.activation(out=th[:], in_=th[:], func=AF.Tanh, scale=0.5), r=THK, w=THK)
            op("act", lambda e, nb=nb: e.activation(out=sg[nb][:], in_=sg[nb][:], func=AF.Silu), r=["sg%d" % nb], w=["sg%d" % nb])
            op("act", lambda e, nb=nb: e.activation(out=sga[nb][:], in_=sga[nb][:], func=AF.Silu), r=["sga%d" % nb], w=["sga%d" % nb])
            yield
            op("act", lambda e: e.activation(out=h_bf[:], in_=qkn[:], func=AF.Square), r=["qkn_a", "qkn_b"], w=["h_bf"])
            op("dve", lambda e: e.tensor_reduce(out=ss2[:], in_=h_bf[:].rearrange("p (g d) -> p g d", d=64), axis=AX.X, op=ALU.add), r=["h_bf"], w=["ss2v"])
            op("act", lambda e: e.activation(out=rs2[:], in_=ss2[:], func=AF.Ln, scale=1.0 / 64, bias=EPS), r=["ss2v"], w=["rs2"])
            op("act", lambda e: e.activation(out=rs2[:], in_=rs2[:], func=AF.Exp, scale=-0.5), r=["rs2"], w=["rs2"])
            op("dve", lambda e: e.tensor_tensor(out=qkn[:].rearrange("p (g d) -> p g d", d=64), in0=qkn[:].rearrange("p (g d) -> p g d", d=64),
                                                in1=rs2[:].unsqueeze(2).to_broadcast([128, 16, 64]), op=ALU.mult), r=["rs2", "qkn_a", "qkn_b"], w=["qkn_a", "qkn_b"])
            yield

        def hgrn_pre(job):
            n, pi, sq, l, t = job["n"], job["pi"], job["sq"], job["l"], job["t"]
            nb = n % 2
            isb = i_sb[nb]
            ik = "i_sb%d" % nb
            D_ = (dbg is not None and dbg == (sq, l, t))
            for h in range(4):
                hs = slice(h * 128, (h + 1) * 128)
                op("dve", lambda e, h=h, hs=hs, l=l: e.tensor_scalar(out=ke[:, hs], in0=th[:, hs], scalar1=cst[:, l, 2, h:h + 1], scalar2=cst[:, l, 0, h:h + 1],
                                                                     op0=ALU.mult, op1=ALU.add), r=["th_%d" % h] + CST, w=["kk_%d" % h])
            for h in range(4):
                hs = slice(h * 128, (h + 1) * 128)
                op("dve", lambda e, h=h, hs=hs, l=l: e.tensor_scalar(out=fb[:, hs], in0=th[:, hs], scalar1=cst[:, l, 0, h:h + 1], scalar2=cst[:, l, 1, h:h + 1],
                                                                     op0=ALU.mult, op1=ALU.add), r=["th_%d" % h] + CST, w=["fb_%d" % h])
            yield
            op("act", lambda e: e.activation(out=fb[:], in_=fb[:], func=AF.Ln), r=FBK, w=FBK)
            op("dve", lambda e: e.tensor_tensor_scan(out=bl[:], data0=rmask[:], data1=fb[:], initial=0.0, op0=ALU.mult, op1=ALU.add), r=FBK + ["rmask"], w=["bl"])
            if D_:
                dump("d_bl", bl[:], ["bl"])
            yield
            op("act", lambda e: e.activation(out=eb[:], in_=bl[:], func=AF.Exp), r=["bl"], w=["eb"])
            op("act", lambda e: e.activation(out=fb[:], in_=bl[:], func=AF.Exp, scale=-1.0), r=["bl"], w=FBK)
            op("dve", lambda e: e.tensor_tensor(out=qe[:], in0=qf[:], in1=eb[:], op=ALU.mult), r=["qf", "eb"], w=["qe"])
            op("dve", lambda e: e.tensor_tensor(out=ke[:], in0=ke[:], in1=fb[:], op=ALU.mult), r=KKK + FBK, w=KKK)
            if D_:
                dump("d_qe", qe[:], ["qe"])
                dump("d_ke", ke[:], KKK)
                dump("d_i", isb[:], [ik])
            yield

        def hgrn(job):
            n, pi, sq, l, t = job["n"], job["pi"], job["sq"], job["l"], job["t"]
            nb = n % 2
            isb = i_sb[nb]
            ik = "i_sb%d" % nb
            D_ = (dbg is not None and dbg == (sq, l, t))
            if t == 0:
                op("pool", lambda e: e.memset(S_sb[:], 0.0), w=["S"])
            for h in range(4):
                hs = slice(h * 128, (h + 1) * 128)
                op("pe", lambda e, hs=hs: e.transpose(Bb[0][:, hs], ke[:, hs], ident[:]), r=KKK + ["ident"], w=["B0"])
            op("dve", lambda e: e.tensor_copy(out=keT[:], in_=Bb[0][:, 0:512]), r=["B0"], w=["keT"])
            for h in range(4):
                hs = slice(h * 128, (h + 1) * 128)
                op("pe", lambda e, hs=hs, h=h: e.matmul(B[1][:, hs], lhsT=ke[:, hs], rhs=qe[:, hs], start=(h == 0), stop=True, skip_group_check=True),
                   r=KKK + ["qe"], w=["B1"])
            op("dve", lambda e: e.copy_predicated(out=A_sb[:], mask=amask[:], data=B[1][:]), r=["B1", "amask", "A_sb"], w=["A_sb"])
            if D_:
                dump("d_A", A_sb[:], ["A_sb"])
            yield
            for h in range(4):
                hs = slice(h * 128, (h + 1) * 128)
                op("pe", lambda e, hs=hs, h=h: e.matmul(B[2][:, hs], lhsT=isb[:, hs], rhs=A_sb[:, hs], start=(h == 0), stop=False, skip_group_check=True),
                   r=[ik, "A_sb"], w=["B2"])
            for j in range(4):
                for h in range(4):
                    js = slice(h * 128 + 32 * j, h * 128 + 32 * j + 32)
                    op("pe", lambda e, h=h, js=js, j=j: e.matmul(B[2][:, js], lhsT=S_sb[:, h, :], rhs=qe[:, js], start=False, stop=(j == 3), skip_group_check=True),
                       r=["S", "qe"], w=["B2"])
                for h in range(4):
                    hs = slice(h * 128, (h + 1) * 128)
                    op("pe", lambda e, h=h, hs=hs: e.matmul(B[1][:, hs], lhsT=ident[:], rhs=S_sb[:, h, :], start=(h == 0), stop=False, skip_group_check=True),
                       r=["S", "ident"], w=["B1"])
                for h in range(4):
                    hs = slice(h * 128, (h + 1) * 128)
                    op("pe", lambda e, h=h, hs=hs, j=j: e.matmul(B[1][:, hs], lhsT=keT[32 * j:32 * j + 32, hs], rhs=isb[32 * j:32 * j + 32, hs], start=False, stop=True,
                                                                skip_group_check=True, tile_position=(32 * j, 0)), r=["keT", ik], w=["B1"])
                dec = eb[:].rearrange("p (h t) -> p h t", h=4)[:, :, 32 * j + 31:32 * j + 32].to_broadcast([128, 4, 128])
                op("dve", lambda e, dec=dec: e.tensor_tensor(out=S_sb[:], in0=B[1][:].rearrange("p (h t) -> p h t", h=4), in1=dec, op=ALU.mult),
                   r=["B1", "eb"], w=["S"])
                yield
            if D_:
                dump("d_S", S_sb[:].rearrange("p h t -> p (h t)"), ["S"])
            op("act", lambda e: e.activation(out=qe[:], in_=B[2][:], func=AF.Square), r=["B2"], w=["qe"])
            op("pe", lambda e: e.matmul(B[1][:], lhsT=onesb[:], rhs=qe[:], start=True, stop=True), r=["qe", "onesb"], w=["B1"])
            op("act", lambda e: e.activation(out=bl[:], in_=B[1][:], func=AF.Ln, scale=1.0 / 128, bias=EPS), r=["B1"], w=["bl"])
            op("act", lambda e: e.activation(out=bl[:], in_=bl[:], func=AF.Exp, scale=-0.5), r=["bl"], w=["bl"])
            yield
            op("dve", lambda e, l=l: e.scalar_tensor_tensor(out=fb[:], in0=B[2][:], scalar=hgw[:, l:l + 1], in1=bl[:], op0=ALU.mult, op1=ALU.mult),
               r=["B2", "hgw", "bl"], w=FBK)
            op("dve", lambda e, nb=nb: e.tensor_tensor(out=mixT[:, 0:4, :].rearrange("p c t -> p (c t)"), in0=fb[:], in1=sg[nb][:], op=ALU.mult), r=FBK + ["sg%d" % nb], w=["mixT_h"])
            yield

        def moba_pre(job):
            n, pi, sq, l, t = job["n"], job["pi"], job["sq"], job["l"], job["t"]
            nb_ = n % 2
            c = t // 2
            D_ = (dbg is not None and dbg == (sq, l, t))
            for i8 in range(8):
                op("pe", lambda e, i8=i8: e.transpose(Bb[0][:, i8 * 128:(i8 + 1) * 128], qkn[:, i8 * 128:(i8 + 1) * 128], ident[:]),
                   r=["qkn_a", "qkn_b", "ident"], w=["B0"])
            qz4 = qTz[:].rearrange("p (c two) t -> p c two t", two=2)
            op("dve", lambda e, l=l: e.tensor_scalar(out=qz4[0:64, :, 0, :], in0=Bb[0][0:64, 0:512].rearrange("p (c t) -> p c t", c=4), scalar1=qg2[0:64, l:l + 1], scalar2=None, op0=ALU.mult),
               r=["B0", "qg2"], w=["qTz_e"])
            op("dve", lambda e, l=l: e.tensor_scalar(out=qz4[64:128, :, 1, :], in0=Bb[0][64:128, 0:512].rearrange("p (c t) -> p c t", c=4), scalar1=qg2[64:128, l:l + 1], scalar2=None, op0=ALU.mult),
               r=["B0", "qg2"], w=["qTz_o"])
            op("dve", lambda e, l=l, t=t: e.tensor_scalar(out=KT_sb[:, :, t * 128:(t + 1) * 128], in0=Bb[0][:, 512:1024].rearrange("p (c t) -> p c t", c=4),
                                                          scalar1=kg2[:, l:l + 1], scalar2=None, op0=ALU.mult), r=["B0", "kg2a", "kg2b"], w=["KT_%d" % t])
            QZ = ["qTz_e", "qTz_o", "qTz0"]
            if t % 2 == 1:
                op("dve", lambda e, c=c: e.tensor_reduce(out=kms[:], in_=KT_sb[:, :, c * 256:(c + 1) * 256], axis=AX.X, op=ALU.add),
                   r=["KT_%d" % (t - 1), "KT_%d" % t], w=["kms"])
                op("dve", lambda e, c=c: e.tensor_scalar(out=KM[0:64, :, c:c + 1], in0=kms[0:64, :].unsqueeze(2), scalar1=1.0 / 256, scalar2=None, op0=ALU.mult),
                   r=["kms", "KM"], w=["KM"])
                op("dve", lambda e, c=c: e.tensor_scalar(out=KM[64:128, :, 8 + c:9 + c], in0=kms[64:128, :].unsqueeze(2), scalar1=1.0 / 256, scalar2=None, op0=ALU.mult),
                   r=["kms", "KM"], w=["KM"])
            if D_:
                dump("d_qT", qTz[:, 0:4, :].rearrange("p c t -> p (c t)"), QZ)
                dump("d_kt", KT_sb[:, :, t * 128:(t + 1) * 128], ["KT_%d" % t])
            yield
            yield
            yield
            op("pool", lambda e: e.memset(selbp[:, :, 0:8], 0.0), w=["selbp"])
            op("pool", lambda e, t=t: e.tensor_copy(out=selbp[:, :, 8:12], in_=ctab[:, t, :, :]), r=["ctab"], w=["selbp_c"])
            if c >= 4:
                for p in range(4):
                    op("pe", lambda e, p=p: e.matmul(B[3][:, p * 16:(p + 1) * 16], lhsT=qTz[:, 2 * p, :], rhs=KM[:, p, :], start=(p == 0), stop=False, skip_group_check=True),
                       r=QZ + ["KM"], w=["B3"])
                    op("pe", lambda e, p=p: e.matmul(B[3][:, p * 16:(p + 1) * 16], lhsT=qTz[:, 2 * p + 1, :], rhs=KM[:, p, :], start=False, stop=True, skip_group_check=True),
                       r=QZ + ["KM"], w=["B3"])
                op("dve", lambda e: e.tensor_copy(out=g_sb[:], in_=B[3][:, 0:64]), r=["B3"], w=["g_sb"])
                g3 = g_sb[:].rearrange("p (h n) -> p h n", h=8)[:, :, 0:c]
                cmp4 = cmp[:, 0:8 * c * c].rearrange("p (h n m) -> p h n m", h=8, n=c)
                op("dve", lambda e, g3=g3, cmp4=cmp4, c=c: e.tensor_tensor(out=cmp4, in0=g3.unsqueeze(2).to_broadcast([128, 8, c, c]),
                                                                           in1=g3.unsqueeze(3).to_broadcast([128, 8, c, c]), op=ALU.is_gt), r=["g_sb"], w=["oa_a", "oa_b"])
                rk3 = rank[:].rearrange("p (h n) -> p h n", h=8)[:, :, 0:c]
                op("dve", lambda e, rk3=rk3, cmp4=cmp4: e.tensor_reduce(out=rk3, in_=cmp4, axis=AX.X, op=ALU.add), r=["oa_a", "oa_b"], w=["rank"])
                op("dve", lambda e, rk3=rk3, c=c: e.tensor_scalar(out=selbp[:, :, 0:c], in0=rk3, scalar1=2.5, scalar2=NEG, op0=ALU.is_gt, op1=ALU.mult),
                   r=["rank", "selbp"], w=["selbp"])
                if D_:
                    dump("d_g", g_sb[:], ["g_sb"])
            if D_:
                dump("d_selbp", selbp[:].rearrange("p h n -> p (h n)"), ["selbp", "selbp_c"])
            yield
            yield
            for hh in range(2):
                op("pe", lambda e, hh=hh: e.transpose(Bb[0][:, hh * 128:(hh + 1) * 128], selbp[:].rearrange("p h n -> p (h n)")[:, hh * 128:(hh + 1) * 128], ident[:]),
                   r=["selbp", "selbp_c", "ident"], w=["B0"])
            op("dve", lambda e: e.tensor_tensor(out=selbTz[:].rearrange("p (hh g) t -> p hh g t", g=4),
                                                in0=Bb[0][:, 0:256].rearrange("p (hh t) -> p hh t", hh=2).unsqueeze(2).to_broadcast([128, 2, 4, 128]),
                                                in1=gmask[:].unsqueeze(1).unsqueeze(3).to_broadcast([128, 2, 4, 128]), op=ALU.mult),
               r=["B0", "gmask"], w=["selbTz"])
            yield

        def moba(job):
            n, pi, sq, l, t = job["n"], job["pi"], job["sq"], job["l"], job["t"]
            nb_ = n % 2
            c = t // 2
            D_ = (dbg is not None and dbg == (sq, l, t))
            QZ = ["qTz_e", "qTz_o", "qTz0"]
            if t == 0 and n > 0:
                op("pool", lambda e: e.tensor_copy(out=V_sb[:, 0, :], in_=vst[:]), r=["vst"], w=["V_0"])
            groups = []
            for rnd in range(4):
                items = [(rnd, kt) for kt in range(t + 1)]
                for b0 in range(0, len(items), 2):
                    groups.append([rnd, items[b0:b0 + 2], False])
                groups[-1][2] = True
            bank_of = {}
            pv_next = [0]

            def qk(gi):
                rnd, grp, _ = groups[gi]
                pool_ = [4, 5] + ([3, 7] if job.get("a_done") else [])
                busy = set(bank_of[g2] for g2 in bank_of if g2 >= pv_next[0])
                bi = [b_ for b_ in pool_ if b_ not in busy][0]
                bank_of[gi] = bi
                bk = "B%d" % bi
                for si, (p, kt) in enumerate(grp):
                    sl = slice(si * 256, (si + 1) * 256)
                    op("pe", lambda e, bi=bi, sl=sl, p=p, kt=kt, si=si: e.matmul(B[bi][:, sl], lhsT=KT_sb[:, p, kt * 128:(kt + 1) * 128],
                                                                             rhs=qTz[:, 2 * p:2 * p + 2, :].rearrange("p a t -> p (a t)"),
                                                                             start=(si == 0), stop=False, skip_group_check=True),
                       r=["KT_%d" % kt] + QZ, w=[bk])
                    op("pe", lambda e, bi=bi, sl=sl, p=p, kt=kt, t=t: e.matmul(B[bi][:, sl], lhsT=Lk[:, kt, :], rhs=selbTz[:, 2 * p:2 * p + 2, :].rearrange("p a t -> p (a t)"),
                                                                           start=False, stop=(kt != t), skip_group_check=True),
                       r=["Lk", "selbTz"], w=[bk])
                    if kt == t:
                        op("pe", lambda e, bi=bi, sl=sl: e.matmul(B[bi][:, sl], lhsT=ident[:], rhs=cmask2[:], start=False, stop=True, skip_group_check=True),
                           r=["ident", "cmask2"], w=[bk])

            def ex(gi):
                rnd, grp, _ = groups[gi]
                bi = bank_of[gi]
                bk = "B%d" % bi
                pt = PT[gi % 2]
                ptk = "PT%d" % (gi % 2)
                n = len(grp) * 256
                op("act", lambda e, bi=bi, pt=pt, n=n: e.activation(out=pt[:, 0:n], in_=B[bi][:, 0:n], func=AF.Exp), r=[bk], w=[ptk])

            acc_of = {}

            def pv(gi):
                rnd, grp, _ = groups[gi]
                if rnd not in acc_of:
                    acc_of[rnd] = 1 if (rnd % 2 == 1 and job.get("h_done")) else 6
                ab = acc_of[rnd]
                abk = "B%d" % ab
                pt = PT[gi % 2]
                ptk = "PT%d" % (gi % 2)
                for si, (p, kt) in enumerate(grp):
                    sl = slice(si * 256, (si + 1) * 256)
                    op("pe", lambda e, kt=kt, p=p, pt=pt, sl=sl, t=t, ab=ab: e.matmul(B[ab][:, 0:256], lhsT=V_sb[:, kt, p * 128:(p + 1) * 128], rhs=pt[:, sl],
                                                                                  start=(kt == 0), stop=(kt == t), skip_group_check=True),
                       r=["V_%d" % kt, ptk], w=[abk])
                    op("pe", lambda e, kt=kt, pt=pt, sl=sl, t=t, ab=ab: e.matmul(B[ab][:, 256:512], lhsT=onesb[:], rhs=pt[:, sl],
                                                                             start=False, stop=(kt == t), skip_group_check=True),
                       r=["onesb", ptk], w=[abk])

            def norm(p):
                ab = acc_of[p]
                abk = "B%d" % ab
                op("dve", lambda e, ab=ab: e.reciprocal(out=rdn[0:64, 0, :], in_=B[ab][0:64, 256:384]), r=[abk], w=["rdn_a"])
                op("dve", lambda e, ab=ab: e.reciprocal(out=rdn[64:128, 0, :], in_=B[ab][64:128, 384:512]), r=[abk], w=["rdn_b"])
                op("dve", lambda e, ab=ab: e.tensor_copy(out=oa[0:64, 0, :], in_=B[ab][0:64, 0:128]), r=[abk], w=["oa_a"])
                op("dve", lambda e, ab=ab: e.tensor_copy(out=oa[64:128, 0, :], in_=B[ab][64:128, 128:256]), r=[abk], w=["oa_b"])
                op("dve", lambda e: e.tensor_tensor(out=oa[:, 0, :], in0=oa[:, 0, :], in1=rdn[:, 0, :], op=ALU.mult), r=["oa_a", "oa_b", "rdn_a", "rdn_b"], w=["oa_a", "oa_b"])
                op("dve", lambda e, p=p: e.tensor_tensor(out=mixT[:, 4 + p, :], in0=oa[:, 0, :], in1=sga[nb_][:, p * 128:(p + 1) * 128], op=ALU.mult),
                   r=["oa_a", "oa_b", "sga%d" % nb_], w=["mixT_a%d" % p])

            ng = len(groups)
            qk_next = [0]

            def fill():
                depth = 2 if job.get("a_done") else 1
                while qk_next[0] < ng and qk_next[0] <= pv_next[0] + depth:
                    qk(qk_next[0])
                    qk_next[0] += 1
                if qk_next[0] == ng:
                    job["qk_done"] = True
            fill()
            ex(0)
            for gi in range(ng):
                pv_next[0] = gi
                fill()
                if gi + 1 < ng:
                    ex(gi + 1)
                pv(gi)
                pv_next[0] = gi + 1
                if groups[gi][2]:
                    norm(groups[gi][0])
                yield
            if D_:
                dump("d_mixT", mixT[:].rearrange("p c t -> p (c t)"), ["mixT_h", "mixT_a0", "mixT_a1", "mixT_a2", "mixT_a3"])
            yield


        def stageD(job):
            n, pi, sq, l, t = job["n"], job["pi"], job["sq"], job["l"], job["t"]
            xk = "x_%d" % t
            nxt = passes[pi + 1] if pi + 1 < len(passes) else None
            for hf in range(2):
                bi = 1 + hf
                for cc in range(8):
                    op("pe", lambda e, bi=bi, cc=cc, hf=hf: e.matmul(B[bi][:], lhsT=mixT[:, cc, :], rhs=wo_sb[:, cc, hf * 512:(hf + 1) * 512], start=(cc == 0), stop=(cc == 7)),
                       r=["mixT_h", "mixT_a0", "mixT_a1", "mixT_a2", "mixT_a3", "wo_%d" % cc], w=["B%d" % bi])
                op("dve", lambda e, bi=bi, hf=hf, t=t: e.tensor_tensor(out=x_sb[:, t, hf * 512:(hf + 1) * 512], in0=B[bi][:], in1=x_sb[:, t, hf * 512:(hf + 1) * 512], op=ALU.add),
                   r=["B%d" % bi, xk], w=[xk])
                yield
            if t == ntiles - 1 and nxt is not None and nxt[1] != l:
                load_w_out(nxt[1])
            if l == layers[-1]:
                out_dmas.append(dma("sp", lambda e, t=t, sq=sq: e.dma_start(out=y_d[sq, t * 128:(t + 1) * 128, :], in_=x_sb[:, t, :]), r=[xk], w=["y_%d_%d" % (sq, t)]))

        def run_all(g):
            for _ in g:
                pass

        run_all(stageA(jobs[0]))
        run_all(hgrn_pre(jobs[0]))
        run_all(moba_pre(jobs[0]))
        for n, job in enumerate(jobs):
            if n > 0 and job["t"] == 0:
                s.new_epoch()
            gens = {"H": hgrn(job), "M": moba(job)}
            has_next = n + 1 < len(jobs)
            if has_next:
                gens["A"] = stageA(jobs[n + 1])
            hp = mp = dstart = False
            if not has_next:
                job["a_done"] = True
            while gens:
                if "A" not in gens:
                    job["a_done"] = True
                if "H" not in gens:
                    job["h_done"] = True
                for key in list(gens):
                    try:
                        next(gens[key])
                        if key == "M" and ("H" in gens or "A" in gens) and job["t"] >= 4:
                            next(gens[key])
                    except StopIteration:
                        del gens[key]
                if not dstart and "H" not in gens and "M" not in gens:
                    gens["D"] = stageD(job)
                    dstart = True
                if has_next:
                    if not hp and "H" not in gens and "A" not in gens:
                        gens["HP"] = hgrn_pre(jobs[n + 1])
                        hp = True
                    if not mp and "A" not in gens and job.get("qk_done"):
                        gens["MP"] = moba_pre(jobs[n + 1])
                        mp = True
            if has_next:
                if not hp:
                    run_all(hgrn_pre(jobs[n + 1]))
                if not mp:
                    run_all(moba_pre(jobs[n + 1]))
            if not dstart:
                run_all(stageD(job))
        s.finish_wait("sp", out_dmas)
        s.emit(st)
    return nc


_PARAM_KEYS = ["norm_w", "w_in", "hg_lb_logits", "hg_norm_w", "q_norm_w", "k_norm_w", "w_out"]


def _launch(nc, x, params, consts, n_cores=8):
    in_maps = []
    for ci in range(n_cores):
        m = {"x": np.ascontiguousarray(x[ci * NSEQ:(ci + 1) * NSEQ])}
        m.update(params)
        m.update(consts)
        in_maps.append(m)
    res = run_bass_kernel_spmd(nc, in_maps, core_ids=list(range(n_cores)))
    return np.concatenate([r["y"] for r in res.results], axis=0)


FUSED = True


def kernel(x, norm_w, w_in, hg_lb_logits, hg_norm_w, q_norm_w, k_norm_w, w_out):
    x = np.ascontiguousarray(np.asarray(x, dtype=np.float32))
    params = dict(norm_w=norm_w, w_in=w_in, hg_lb_logits=hg_lb_logits, hg_norm_w=hg_norm_w,
                  q_norm_w=q_norm_w, k_norm_w=k_norm_w, w_out=w_out)
    params = {k: np.ascontiguousarray(np.asarray(v, dtype=np.float32)) for k, v in params.items()}
    consts = _consts()
    if FUSED:
        nc = _build([0, 1], 0)
        return _launch(nc, x, params, consts)
    y = x
    for l in range(2):
        nc = _build([l], 0)
        y = _launch(nc, y, params, consts)
    return y
```

```python
import numpy as np
import ml_dtypes
from contextlib import ExitStack
import concourse.bass as bass
import concourse.mybir as mybir
from concourse.bass_utils import run_bass_kernel_spmd

F32 = mybir.dt.float32
BF16 = mybir.dt.bfloat16
U32 = mybir.dt.uint8
ALU = mybir.AluOpType
AF = mybir.ActivationFunctionType
AX = mybir.AxisListType

NT = 16
NSEQ = 2
EPS = 1e-6
NEG = -30000.0


class Sched:
    REAL = ("pe", "act", "dve", "pool", "sp")

    def __init__(self, nc, n_dma_sems=8):
        self.nc = nc
        self.ops = {e: [] for e in self.REAL}
        self.nops = {}
        self.clock = {e: {} for e in self.REAL}
        self.opclock = {}
        self.lastw = {}
        self.readers = {}
        self.signaled = set()
        self.n_dma_sems = n_dma_sems
        self.dma_rr = {"sp": 0, "pool": 0, "act": 0}
        self.epochs = {e: [0] for e in self.REAL}
        self.strict = True

    def new_epoch(self):
        for e in self.REAL:
            self.epochs[e].append(len(self.ops[e]))

    def _deps(self, eng, r, w):
        raw, other = set(), set()
        for k in r:
            if k in self.lastw:
                raw.add(self.lastw[k])
        for k in w:
            if k in self.lastw:
                other.add(self.lastw[k])
            for d in self.readers.get(k, ()):
                other.add(d)
        deps = set(raw)
        for d in other:
            if d[0] != eng or (self.strict and eng != "pe"):
                deps.add(d)
        return deps

    def _apply_waits(self, eng, deps):
        clk = self.clock[eng]
        waits = []
        best = {}
        for (e2, i2) in deps:
            if best.get(e2, -1) < i2:
                best[e2] = i2
        for e2, i2 in best.items():
            if clk.get(e2, -1) >= i2:
                continue
            waits.append((e2, i2))
            self.signaled.add((e2, i2))
            oc = self.opclock.get((e2, i2), {})
            for k, v in oc.items():
                if clk.get(k, -1) < v:
                    clk[k] = v
            if clk.get(e2, -1) < i2:
                clk[e2] = i2
        return waits

    def _record(self, me, r, w):
        for k in r:
            self.readers.setdefault(k, set()).add(me)
        for k in w:
            self.lastw[k] = me
            self.readers[k] = set()

    def op(self, eng, fn, r=(), w=()):
        deps = self._deps(eng, r, w)
        waits = self._apply_waits(eng, deps)
        idx = len(self.ops[eng])
        self.ops[eng].append(dict(fn=fn, waits=waits, dma=None))
        snap = dict(self.clock[eng])
        snap[eng] = idx
        self.opclock[(eng, idx)] = snap
        me = (eng, idx)
        self._record(me, r, w)
        return me

    def dma(self, q, fn, r=(), w=()):
        slot = self.dma_rr[q]
        self.dma_rr[q] = (slot + 1) % (4 if q == "pool" else self.n_dma_sems)
        pe = "dma_%s_%d" % (q, slot)
        pidx = self.nops.get(pe, 0)
        self.nops[pe] = pidx + 1
        deps = self._deps(pe, r, w)
        if pidx > 0:
            deps.add((pe, pidx - 1))
        waits = self._apply_waits(q, deps)
        self.ops[q].append(dict(fn=fn, waits=waits, dma=(pe, pidx)))
        snap = dict(self.clock[q])
        snap[pe] = pidx
        self.opclock[(pe, pidx)] = snap
        me = (pe, pidx)
        self._record(me, r, w)
        return me

    def finish_wait(self, eng, deps):
        waits = self._apply_waits(eng, set(deps))
        self.ops[eng].append(dict(fn=None, waits=waits, dma=None))

    def emit(self, stack):
        nc = self.nc
        sems = {}
        val = {}
        for e in self.REAL:
            bounds = self.epochs[e] + [len(self.ops[e]) + 1]
            ep = 0
            c = 0
            cur = None
            for i in range(len(self.ops[e])):
                while i >= bounds[ep + 1]:
                    ep += 1
                    c = 0
                    cur = None
                if (e, i) in self.signaled:
                    if cur is None:
                        cur = stack.enter_context(nc.semaphore("s_%s_%d" % (e, ep)))
                    c += 1
                    val[(e, i)] = (cur, c)
        for pe, n in self.nops.items():
            sm = stack.enter_context(nc.semaphore("s_" + pe))
            sems[pe] = sm
            for i in range(n):
                val[(pe, i)] = (sm, 16 * (i + 1))
        block = stack.enter_context(nc.Block())
        binders = {"pe": block.tensor, "act": block.scalar, "dve": block.vector,
                   "pool": block.gpsimd, "sp": block.sync}

        def mk(e):
            ops = self.ops[e]

            def body(engine):
                for i, o in enumerate(ops):
                    for d in o["waits"]:
                        sm, v = val[d]
                        engine.wait_ge(sm, v)
                    if o["fn"] is None:
                        continue
                    ins = o["fn"](engine)
                    if o["dma"] is not None:
                        ins.then_inc(sems[o["dma"][0]], 16)
                    elif (e, i) in self.signaled:
                        ins.then_inc(val[(e, i)][0], 1)
            return body

        for e in self.REAL:
            if self.ops[e]:
                binders[e](mk(e))


def _consts():
    c = {}
    c["ident"] = np.eye(128, dtype=np.float32).astype(ml_dtypes.bfloat16)
    c["onesb"] = np.ones((128, 128), np.float32).astype(ml_dtypes.bfloat16)
    rm = np.ones((128, 512), np.float32)
    rm[:, ::32] = 0
    c["rmask"] = rm.astype(ml_dtypes.bfloat16)
    s = np.arange(128)[:, None]
    t = np.arange(128)[None, :]
    am = ((s // 32 == t // 32) & (t >= s)).astype(np.uint8)
    c["amask"] = np.ascontiguousarray(np.tile(am, (1, 4)))
    c["cmask"] = np.where(s > t, NEG, 0.0).astype(np.float32).astype(ml_dtypes.bfloat16)
    lk = np.zeros((128, NT, 128), np.float32)
    for g in range(4):
        for kt in range(NT):
            lk[32 * g + kt // 2, kt, :] = 1.0
            lk[32 * g + 8, kt, :] = np.arange(128)
            lk[32 * g + 9, kt, :] = 1.0
            lk[32 * g + 10, kt, :] = 1.0
            lk[32 * g + 11, kt, :] = 128.0 * kt
    c["lk"] = lk.astype(ml_dtypes.bfloat16)
    c["cmask2"] = np.ascontiguousarray(np.concatenate([c["cmask"], c["cmask"]], axis=1))
    gm = np.zeros((128, 4), np.float32)
    for g in range(4):
        gm[32 * g:32 * g + 32, g] = 1.0
    c["gmask"] = gm
    ct = np.zeros((128, NT, 8, 4), np.float32)
    for h in range(8):
        sl = 2.0 ** (-(h + 1))
        for qt in range(NT):
            ct[:, qt, h, 0] = sl
            ct[:, qt, h, 1] = -sl * np.arange(128)
            ct[:, qt, h, 2] = -sl * 128.0 * qt
            ct[:, qt, h, 3] = sl
    c["ctab"] = ct.astype(ml_dtypes.bfloat16)
    return c


def _build(layers, first_layer_of_module, nseq=NSEQ, ntiles=NT, dbg=None):
    nc = bass.Bass("TRN2", target_bir_lowering=False, dynamic_dma_scratch_size=2048)
    D = nc.dram_tensor
    x_d = D("x", [NSEQ, 2048, 1024], F32, kind="ExternalInput").ap()
    y_d = D("y", [NSEQ, 2048, 1024], F32, kind="ExternalOutput").ap()
    win_d = D("w_in", [2, 1024, 4096], F32, kind="ExternalInput").ap()
    wout_d = D("w_out", [2, 1024, 1024], F32, kind="ExternalInput").ap()
    nw_d = D("norm_w", [2, 1024], F32, kind="ExternalInput").ap()
    lb_d = D("hg_lb_logits", [2, 512], F32, kind="ExternalInput").ap()
    hgw_d = D("hg_norm_w", [2, 128], F32, kind="ExternalInput").ap()
    qw_d = D("q_norm_w", [2, 64], F32, kind="ExternalInput").ap()
    kw_d = D("k_norm_w", [2, 64], F32, kind="ExternalInput").ap()
    ident_d = D("ident", [128, 128], BF16, kind="ExternalInput").ap()
    ones_d = D("onesb", [128, 128], BF16, kind="ExternalInput").ap()
    rmask_d = D("rmask", [128, 512], BF16, kind="ExternalInput").ap()
    amask_d = D("amask", [128, 512], U32, kind="ExternalInput").ap()
    cmask_d = D("cmask", [128, 128], BF16, kind="ExternalInput").ap()
    lk_d = D("lk", [128, NT, 128], BF16, kind="ExternalInput").ap()
    ctab_d = D("ctab", [128, NT, 8, 4], BF16, kind="ExternalInput").ap()
    cmask2_d = D("cmask2", [128, 256], BF16, kind="ExternalInput").ap()
    gmask_d = D("gmask", [128, 4], F32, kind="ExternalInput").ap()
    dbg_d = {}
    if dbg is not None:
        for nm, shp, dt in [("d_hT", [128, 1024], BF16), ("d_qe", [128, 512], BF16), ("d_ke", [128, 512], BF16),
                            ("d_A", [128, 512], BF16), ("d_S", [128, 512], BF16), ("d_mixT", [128, 1024], BF16),
                            ("d_qT", [128, 512], BF16), ("d_g", [128, 64], F32), ("d_selbp", [128, 256], BF16),
                            ("d_bl", [128, 512], F32), ("d_i", [128, 512], BF16), ("d_kt", [128, 512], BF16)]:
            dbg_d[nm] = D(nm, shp, dt, kind="ExternalOutput").ap()

    with ExitStack() as st:
        T = lambda name, shape, dt: st.enter_context(nc.sbuf_tensor(name, shape, dt))
        x_sb = T("x_sb", [128, NT, 1024], F32)
        w_sb = T("w_sb", [128, 8, 4096], BF16)
        wo_sb = T("wo_sb", [128, 8, 1024], BF16)
        KT_sb = T("KT_sb", [128, 4, 2048], BF16)
        V_sb = T("V_sb", [128, NT, 512], BF16)
        h_bf = T("h_bf", [128, 1024], BF16)
        hT = T("hT", [128, 8, 128], BF16)
        qf = T("qf", [128, 512], BF16)
        fb = T("fb", [128, 512], F32)
        th = T("th", [128, 512], F32)
        bl = T("bl", [128, 512], F32)
        eb = T("eb", [128, 512], BF16)
        qe = T("qe", [128, 512], BF16)
        ke = T("ke", [128, 512], BF16)
        keT = T("keT", [128, 512], BF16)
        A_sb = T("A_sb", [128, 512], BF16)
        S_sb = T("S_sb", [128, 4, 128], BF16)
        i_sb = [T("i_sb0", [128, 512], BF16), T("i_sb1", [128, 512], BF16)]
        sg = [T("sg0", [128, 512], BF16), T("sg1", [128, 512], BF16)]
        sga = [T("sga0", [128, 512], BF16), T("sga1", [128, 512], BF16)]
        mixT = T("mixT", [128, 8, 128], BF16)
        qkn = T("qkn", [128, 1024], BF16)
        qTz = T("qTz", [128, 8, 128], BF16)
        vst = T("vst", [128, 512], BF16)
        rdn = T("rdn", [128, 2, 128], F32)
        oa = T("oa", [128, 2, 128], F32)
        cmp = oa[:].rearrange("p a t -> p (a t)").bitcast(BF16)
        g_sb = T("g_sb", [128, 64], F32)
        rank = T("rank", [128, 64], F32)
        selbp = T("selbp", [128, 8, 32], BF16)
        selbTz = T("selbTz", [128, 8, 128], BF16)
        cmask2 = T("cmask2_sb", [128, 256], BF16)
        gmask = T("gmask_sb", [128, 4], F32)
        PT = [T("PT0", [128, 512], BF16), T("PT1", [128, 512], BF16)]
        ident = T("ident_sb", [128, 128], BF16)
        onesb = T("ones_sb", [128, 128], BF16)
        rmask = T("rmask_sb", [128, 512], BF16)
        amask = T("amask_sb", [128, 512], U32)
        cmask = T("cmask_sb", [128, 128], BF16)
        Lk = T("lk_sb", [128, NT, 128], BF16)
        ctab = T("ctab_sb", [128, NT, 8, 4], BF16)
        KM = T("KM", [128, 4, 16], BF16)
        kms = T("kms", [128, 4], F32)
        nwT = T("nwT", [128, 2, 8], F32)
        lbl = T("lbl", [128, 2, 4], F32)
        cst = T("cst", [128, 2, 3, 4], F32)
        hgw = T("hgw", [128, 2], F32)
        qg2 = T("qg2", [128, 2], F32)
        kg2 = T("kg2", [128, 2], F32)
        ss = T("ss", [128, 4], F32)
        ss2 = T("ss2", [128, 16], F32)
        rs2 = T("rs2", [128, 16], F32)
        tmp4 = T("tmp4", [128, 4], F32)
        B = [st.enter_context(nc.psum_tensor("B%d" % i, [128, 512], F32)) for i in range(8)]
        Bb = [b[:].bitcast(BF16) for b in B]

        s = Sched(nc)
        op, dma = s.op, s.dma

        for sbt, drt, key in [(ident, ident_d, "ident"), (onesb, ones_d, "onesb"), (rmask, rmask_d, "rmask"),
                              (amask, amask_d, "amask"), (cmask, cmask_d, "cmask"), (Lk, lk_d, "Lk"), (ctab, ctab_d, "ctab"),
                              (cmask2, cmask2_d, "cmask2"), (gmask, gmask_d, "gmask")]:
            dma("sp", lambda e, a=sbt, b=drt: e.dma_start(out=a[:], in_=b[:]), w=[key])
        with nc.allow_non_contiguous_dma(reason="tiny param loads"):
            dma("sp", lambda e: e.dma_start(out=nwT[:], in_=nw_d.rearrange("l (k p) -> p l k", p=128), allow_slow_non_contiguous=True), w=["nwT"])
            dma("sp", lambda e: e.dma_start(out=lbl[:], in_=lb_d.rearrange("l (h p) -> p l h", p=128), allow_slow_non_contiguous=True), w=["lbl"])
            dma("sp", lambda e: e.dma_start(out=hgw[:], in_=hgw_d.rearrange("l p -> p l"), allow_slow_non_contiguous=True), w=["hgw"])
            dma("sp", lambda e: e.dma_start(out=qg2[0:64, :], in_=qw_d.rearrange("l p -> p l"), allow_slow_non_contiguous=True), w=["qg2a"])
            dma("sp", lambda e: e.dma_start(out=qg2[64:128, :], in_=qw_d.rearrange("l p -> p l"), allow_slow_non_contiguous=True), w=["qg2b"])
            dma("sp", lambda e: e.dma_start(out=kg2[0:64, :], in_=kw_d.rearrange("l p -> p l"), allow_slow_non_contiguous=True), w=["kg2a"])
            dma("sp", lambda e: e.dma_start(out=kg2[64:128, :], in_=kw_d.rearrange("l p -> p l"), allow_slow_non_contiguous=True), w=["kg2b"])
        op("dve", lambda e: e.tensor_scalar(out=qg2[:], in0=qg2[:], scalar1=0.125, scalar2=None, op0=ALU.mult),
           r=["qg2a", "qg2b"], w=["qg2"])
        op("dve", lambda e: e.tensor_tensor(out=tmp4[:], in0=lbl[:, 0, :], in1=lbl[:, 1, :], op=ALU.subtract), r=["lbl"], w=["tmp4"])
        op("act", lambda e: e.activation(out=tmp4[:], in_=tmp4[:], func=AF.Exp), r=["tmp4"], w=["tmp4"])
        op("dve", lambda e: e.tensor_scalar(out=tmp4[:], in0=tmp4[:], scalar1=1.0, scalar2=None, op0=ALU.add), r=["tmp4"], w=["tmp4"])
        op("dve", lambda e: e.reciprocal(out=tmp4[:], in_=tmp4[:]), r=["tmp4"], w=["tmp4"])
        op("dve", lambda e: e.tensor_scalar(out=tmp4[:], in0=tmp4[:], scalar1=1.0 - 1e-6, scalar2=0.0, op0=ALU.min, op1=ALU.max), r=["tmp4"], w=["tmp4"])
        op("dve", lambda e: e.memset(cst[:, 0, 0, :], 0.5), w=["cst0a"])
        op("dve", lambda e: e.memset(cst[:, 0, 1, :], 0.5), w=["cst0b"])
        op("dve", lambda e: e.memset(cst[:, 0, 2, :], -0.5), w=["cst0c"])
        op("dve", lambda e: e.tensor_scalar(out=cst[:, 1, 0, :], in0=tmp4[:], scalar1=-0.5, scalar2=0.5, op0=ALU.mult, op1=ALU.add), r=["tmp4"], w=["cst1a"])
        op("dve", lambda e: e.tensor_scalar(out=cst[:, 1, 1, :], in0=tmp4[:], scalar1=0.5, scalar2=0.5, op0=ALU.mult, op1=ALU.add), r=["tmp4"], w=["cst1b"])
        op("dve", lambda e: e.tensor_scalar(out=cst[:, 1, 2, :], in0=tmp4[:], scalar1=0.5, scalar2=-0.5, op0=ALU.mult, op1=ALU.add), r=["tmp4"], w=["cst1c"])
        CST = ["cst0a", "cst0b", "cst0c", "cst1a", "cst1b", "cst1c"]
        op("pool", lambda e: e.memset(A_sb[:], 0.0), w=["A_sb"])
        op("pool", lambda e: e.memset(selbp[:], 0.0), w=["selbp"])
        op("pool", lambda e: e.memset(KM[:], 0.0), w=["KM"])
        op("pool", lambda e: e.memset(qTz[:], 0.0), w=["qTz0"])

        def load_w_in(l):
            for k in range(8):
                for hf in range(2):
                    dma("pool", lambda e, k=k, hf=hf, l=l: e.dma_start(out=w_sb[:, k, hf * 2048:(hf + 1) * 2048],
                                                                       in_=win_d[l, k * 128:(k + 1) * 128, hf * 2048:(hf + 1) * 2048]),
                        w=["win_%d_%d" % (k, hf)])

        def load_w_out(l):
            for cc in range(8):
                dma("pool", lambda e, cc=cc, l=l: e.dma_start(out=wo_sb[:, cc, :], in_=wout_d[l, cc * 128:(cc + 1) * 128, :]),
                    w=["wo_%d" % cc])

        passes = [(sq, l) for sq in range(nseq) for l in layers]
        load_w_in(passes[0][1])
        load_w_out(passes[0][1])
        out_dmas = []
        FBK = ["fb_%d" % h for h in range(4)]
        KKK = ["kk_%d" % h for h in range(4)]
        THK = ["th_%d" % h for h in range(4)]

        def dump(name, ap, keys):
            out_dmas.append(dma("sp", lambda e, a=ap, n=name: e.dma_start(out=dbg_d[n][:], in_=a), r=keys, w=["dbgout_" + name]))

        jobs = []
        for pi, (sq, l) in enumerate(passes):
            for t in range(ntiles):
                jobs.append(dict(n=len(jobs), pi=pi, sq=sq, l=l, t=t))

        def xload(sq, t):
            dma("sp", lambda e, t=t, sq=sq: e.dma_start(out=x_sb[:, t, :], in_=x_d[sq, t * 128:(t + 1) * 128, :]), w=["x_%d" % t])

        def stageA(job):
            n, pi, sq, l, t = job["n"], job["pi"], job["sq"], job["l"], job["t"]
            nb = n % 2
            D_ = (dbg is not None and dbg == (sq, l, t))
            xk = "x_%d" % t
            nxt = passes[pi + 1] if pi + 1 < len(passes) else None
            if l == layers[0]:
                if t == 0:
                    xload(sq, 0)
                if t + 1 < ntiles:
                    xload(sq, t + 1)
            op("act", lambda e, t=t: e.activation(out=h_bf[:], in_=x_sb[:, t, :], func=AF.Square, accum_out=ss[:, 0:1]), r=[xk], w=["h_bf", "ss"])
            op("act", lambda e: e.activation(out=ss[:, 1:2], in_=ss[:, 0:1], func=AF.Ln, scale=1.0 / 1024, bias=EPS), r=["ss"], w=["ss1"])
            op("act", lambda e: e.activation(out=ss[:, 2:3], in_=ss[:, 1:2], func=AF.Exp, scale=-0.5), r=["ss1"], w=["ss2"])
            op("dve", lambda e, t=t: e.tensor_scalar(out=h_bf[:], in0=x_sb[:, t, :], scalar1=ss[:, 2:3], scalar2=None, op0=ALU.mult), r=[xk, "ss2"], w=["h_bf"])
            for k in range(8):
                op("pe", lambda e, k=k: e.transpose(Bb[0][:, k * 128:(k + 1) * 128], h_bf[:, k * 128:(k + 1) * 128], ident[:]), r=["h_bf", "ident"], w=["B0"])
            op("dve", lambda e, l=l: e.tensor_tensor(out=hT[:], in0=Bb[0].rearrange("p (k t) -> p k t", k=8),
                                                    in1=nwT[:, l, :].unsqueeze(2).to_broadcast([128, 8, 128]), op=ALU.mult), r=["B0", "nwT"], w=["hT"])
            if D_:
                dump("d_hT", hT[:].rearrange("p k t -> p (k t)"), ["hT"])
            yield

            def fm(c0, bi):
                for j in range(4):
                    for k in range(8):
                        cc0 = c0 + j * 128
                        op("pe", lambda e, j=j, k=k, cc0=cc0, bi=bi: e.matmul(B[bi][:, j * 128:(j + 1) * 128], lhsT=w_sb[:, k, cc0:cc0 + 128], rhs=hT[:, k, :],
                                                                            start=(k == 0), stop=(k == 7)),
                           r=["hT", "win_%d_%d" % (k, cc0 // 2048)], w=["B%d" % bi])

            def tm(c0, bi):
                for k in range(8):
                    op("pe", lambda e, k=k, c0=c0, bi=bi: e.matmul(B[bi][:, 0:512], lhsT=hT[:, k, :], rhs=w_sb[:, k, c0:c0 + 512], start=(k == 0), stop=(k == 7)),
                       r=["hT", "win_%d_%d" % (k, c0 // 2048)], w=["B%d" % bi])

            def ev_v(bi):
                if t == 0 and n > 0:
                    op("act", lambda e, bi=bi: e.copy(out=vst[:], in_=B[bi][:]), r=["B%d" % bi], w=["vst"])
                else:
                    op("act", lambda e, t=t, bi=bi: e.copy(out=V_sb[:, t, :], in_=B[bi][:]), r=["B%d" % bi], w=["V_%d" % t])
            glist = [
                (lambda bi: tm(3072, bi), ev_v),
                (lambda bi: tm(1024, bi), lambda bi: op("dve", lambda e, nb=nb, bi=bi: e.tensor_copy(out=i_sb[nb][:], in_=B[bi][:]), r=["B%d" % bi], w=["i_sb%d" % nb])),
                (lambda bi: tm(2048, bi), lambda bi: op("act", lambda e, bi=bi: e.copy(out=qkn[:, 0:512], in_=B[bi][:]), r=["B%d" % bi], w=["qkn_a"])),
                (lambda bi: tm(2560, bi), lambda bi: op("act", lambda e, bi=bi: e.copy(out=qkn[:, 512:1024], in_=B[bi][:]), r=["B%d" % bi], w=["qkn_b"])),
                (lambda bi: fm(1536, bi), lambda bi: op("act", lambda e, nb=nb, bi=bi: e.copy(out=sg[nb][:], in_=B[bi][:]), r=["B%d" % bi], w=["sg%d" % nb])),
                (lambda bi: fm(3584, bi), lambda bi: op("act", lambda e, nb=nb, bi=bi: e.copy(out=sga[nb][:], in_=B[bi][:]), r=["B%d" % bi], w=["sga%d" % nb])),
                (lambda bi: fm(512, bi), lambda bi: op("act", lambda e, bi=bi: e.copy(out=th[:], in_=B[bi][:]), r=["B%d" % bi], w=THK)),
                (lambda bi: fm(0, bi), lambda bi: op("act", lambda e, bi=bi: e.copy(out=qf[:], in_=B[bi][:]), r=["B%d" % bi], w=["qf"])),
            ]
            AB = [3, 7]
            for gk, (mmf, evf) in enumerate(glist):
                mmf(AB[gk % 2])
                if gk > 0:
                    glist[gk - 1][1](AB[(gk - 1) % 2])
                yield
            if t == ntiles - 1 and nxt is not None and nxt[1] != l:
                load_w_in(nxt[1])
            glist[-1][1](AB[(len(glist) - 1) % 2])
            yield
            op("act", lambda e: e.activation(out=qf[:], in_=qf[:], func=AF.Silu), r=["qf"], w=["qf"])
            op("act", lambda e: e.activation(out=th[:], in_=th[:], func=AF.Tanh, scale=0.5), r=THK, w=THK)
            op("act", lambda e, nb=nb: e.activation(out=sg[nb][:], in_=sg[nb][:], func=AF.Silu), r=["sg%d" % nb], w=["sg%d" % nb])
            op("act", lambda e, nb=nb: e.activation(out=sga[nb][:], in_=sga[nb][:], func=AF.Silu), r=["sga%d" % nb], w=["sga%d" % nb])
            yield
            op("act", lambda e: e.activation(out=h_bf[:], in_=qkn[:], func=AF.Square), r=["qkn_a", "qkn_b"], w=["h_bf"])
            op("dve", lambda e: e.tensor_reduce(out=ss2[:], in_=h_bf[:].rearrange("p (g d) -> p g d", d=64), axis=AX.X, op=ALU.add), r=["h_bf"], w=["ss2v"])
            op("act", lambda e: e.activation(out=rs2[:], in_=ss2[:], func=AF.Ln, scale=1.0 / 64, bias=EPS), r=["ss2v"], w=["rs2"])
            op("act", lambda e: e.activation(out=rs2[:], in_=rs2[:], func=AF.Exp, scale=-0.5), r=["rs2"], w=["rs2"])
            op("dve", lambda e: e.tensor_tensor(out=qkn[:].rearrange("p (g d) -> p g d", d=64), in0=qkn[:].rearrange("p (g d) -> p g d", d=64),
                                                in1=rs2[:].unsqueeze(2).to_broadcast([128, 16, 64]), op=ALU.mult), r=["rs2", "qkn_a", "qkn_b"], w=["qkn_a", "qkn_b"])
            yield

        def hgrn_pre(job):
            n, pi, sq, l, t = job["n"], job["pi"], job["sq"], job["l"], job["t"]
            nb = n % 2
            isb = i_sb[nb]
            ik = "i_sb%d" % nb
            D_ = (dbg is not None and dbg == (sq, l, t))
            for h in range(4):
                hs = slice(h * 128, (h + 1) * 128)
                op("dve", lambda e, h=h, hs=hs, l=l: e.tensor_scalar(out=ke[:, hs], in0=th[:, hs], scalar1=cst[:, l, 2, h:h + 1], scalar2=cst[:, l, 0, h:h + 1],
                                                                     op0=ALU.mult, op1=ALU.add), r=["th_%d" % h] + CST, w=["kk_%d" % h])
            for h in range(4):
                hs = slice(h * 128, (h + 1) * 128)
                op("dve", lambda e, h=h, hs=hs, l=l: e.tensor_scalar(out=fb[:, hs], in0=th[:, hs], scalar1=cst[:, l, 0, h:h + 1], scalar2=cst[:, l, 1, h:h + 1],
                                                                     op0=ALU.mult, op1=ALU.add), r=["th_%d" % h] + CST, w=["fb_%d" % h])
            yield
            op("act", lambda e: e.activation(out=fb[:], in_=fb[:], func=AF.Ln), r=FBK, w=FBK)
            op("dve", lambda e: e.tensor_tensor_scan(out=bl[:], data0=rmask[:], data1=fb[:], initial=0.0, op0=ALU.mult, op1=ALU.add), r=FBK + ["rmask"], w=["bl"])
            if D_:
                dump("d_bl", bl[:], ["bl"])
            yield
            op("act", lambda e: e.activation(out=eb[:], in_=bl[:], func=AF.Exp), r=["bl"], w=["eb"])
            op("act", lambda e: e.activation(out=fb[:], in_=bl[:], func=AF.Exp, scale=-1.0), r=["bl"], w=FBK)
            op("dve", lambda e: e.tensor_tensor(out=qe[:], in0=qf[:], in1=eb[:], op=ALU.mult), r=["qf", "eb"], w=["qe"])
            op("dve", lambda e: e.tensor_tensor(out=ke[:], in0=ke[:], in1=fb[:], op=ALU.mult), r=KKK + FBK, w=KKK)
            if D_:
                dump("d_qe", qe[:], ["qe"])
                dump("d_ke", ke[:], KKK)
                dump("d_i", isb[:], [ik])
            yield

        def hgrn(job):
            n, pi, sq, l, t = job["n"], job["pi"], job["sq"], job["l"], job["t"]
            nb = n % 2
            isb = i_sb[nb]
            ik = "i_sb%d" % nb
            D_ = (dbg is not None and dbg == (sq, l, t))
            if t == 0:
                op("pool", lambda e: e.memset(S_sb[:], 0.0), w=["S"])
            for h in range(4):
                hs = slice(h * 128, (h + 1) * 128)
                op("pe", lambda e, hs=hs: e.transpose(Bb[0][:, hs], ke[:, hs], ident[:]), r=KKK + ["ident"], w=["B0"])
            op("dve", lambda e: e.tensor_copy(out=keT[:], in_=Bb[0][:, 0:512]), r=["B0"], w=["keT"])
            for h in range(4):
                hs = slice(h * 128, (h + 1) * 128)
                op("pe", lambda e, hs=hs, h=h: e.matmul(B[1][:, hs], lhsT=ke[:, hs], rhs=qe[:, hs], start=(h == 0), stop=True, skip_group_check=True),
                   r=KKK + ["qe"], w=["B1"])
            op("dve", lambda e: e.copy_predicated(out=A_sb[:], mask=amask[:], data=B[1][:]), r=["B1", "amask", "A_sb"], w=["A_sb"])
            if D_:
                dump("d_A", A_sb[:], ["A_sb"])
            yield
            for h in range(4):
                hs = slice(h * 128, (h + 1) * 128)
                op("pe", lambda e, hs=hs, h=h: e.matmul(B[2][:, hs], lhsT=isb[:, hs], rhs=A_sb[:, hs], start=(h == 0), stop=False, skip_group_check=True),
                   r=[ik, "A_sb"], w=["B2"])
            for j in range(4):
                for h in range(4):
                    js = slice(h * 128 + 32 * j, h * 128 + 32 * j + 32)
                    op("pe", lambda e, h=h, js=js, j=j: e.matmul(B[2][:, js], lhsT=S_sb[:, h, :], rhs=qe[:, js], start=False, stop=(j == 3), skip_group_check=True),
                       r=["S", "qe"], w=["B2"])
                for h in range(4):
                    hs = slice(h * 128, (h + 1) * 128)
                    op("pe", lambda e, h=h, hs=hs: e.matmul(B[1][:, hs], lhsT=ident[:], rhs=S_sb[:, h, :], start=(h == 0), stop=False, skip_group_check=True),
                       r=["S", "ident"], w=["B1"])
                for h in range(4):
                    hs = slice(h * 128, (h + 1) * 128)
                    op("pe", lambda e, h=h, hs=hs, j=j: e.matmul(B[1][:, hs], lhsT=keT[32 * j:32 * j + 32, hs], rhs=isb[32 * j:32 * j + 32, hs], start=False, stop=True,
                                                                skip_group_check=True, tile_position=(32 * j, 0)), r=["keT", ik], w=["B1"])
                dec = eb[:].rearrange("p (h t) -> p h t", h=4)[:, :, 32 * j + 31:32 * j + 32].to_broadcast([128, 4, 128])
                op("dve", lambda e, dec=dec: e.tensor_tensor(out=S_sb[:], in0=B[1][:].rearrange("p (h t) -> p h t", h=4), in1=dec, op=ALU.mult),
                   r=["B1", "eb"], w=["S"])
                yield
            if D_:
                dump("d_S", S_sb[:].rearrange("p h t -> p (h t)"), ["S"])
            op("act", lambda e: e.activation(out=qe[:], in_=B[2][:], func=AF.Square), r=["B2"], w=["qe"])
            op("pe", lambda e: e.matmul(B[1][:], lhsT=onesb[:], rhs=qe[:], start=True, stop=True), r=["qe", "onesb"], w=["B1"])
            op("act", lambda e: e.activation(out=bl[:], in_=B[1][:], func=AF.Ln, scale=1.0 / 128, bias=EPS), r=["B1"], w=["bl"])
            op("act", lambda e: e.activation(out=bl[:], in_=bl[:], func=AF.Exp, scale=-0.5), r=["bl"], w=["bl"])
            yield
            op("dve", lambda e, l=l: e.scalar_tensor_tensor(out=fb[:], in0=B[2][:], scalar=hgw[:, l:l + 1], in1=bl[:], op0=ALU.mult, op1=ALU.mult),
               r=["B2", "hgw", "bl"], w=FBK)
            op("dve", lambda e, nb=nb: e.tensor_tensor(out=mixT[:, 0:4, :].rearrange("p c t -> p (c t)"), in0=fb[:], in1=sg[nb][:], op=ALU.mult), r=FBK + ["sg%d" % nb], w=["mixT_h"])
            yield

        def moba_pre(job):
            n, pi, sq, l, t = job["n"], job["pi"], job["sq"], job["l"], job["t"]
            nb_ = n % 2
            c = t // 2
            D_ = (dbg is not None and dbg == (sq, l, t))
            for i8 in range(8):
                op("pe", lambda e, i8=i8: e.transpose(Bb[0][:, i8 * 128:(i8 + 1) * 128], qkn[:, i8 * 128:(i8 + 1) * 128], ident[:]),
                   r=["qkn_a", "qkn_b", "ident"], w=["B0"])
            qz4 = qTz[:].rearrange("p (c two) t -> p c two t", two=2)
            op("dve", lambda e, l=l: e.tensor_scalar(out=qz4[0:64, :, 0, :], in0=Bb[0][0:64, 0:512].rearrange("p (c t) -> p c t", c=4), scalar1=qg2[0:64, l:l + 1], scalar2=None, op0=ALU.mult),
               r=["B0", "qg2"], w=["qTz_e"])
            op("dve", lambda e, l=l: e.tensor_scalar(out=qz4[64:128, :, 1, :], in0=Bb[0][64:128, 0:512].rearrange("p (c t) -> p c t", c=4), scalar1=qg2[64:128, l:l + 1], scalar2=None, op0=ALU.mult),
               r=["B0", "qg2"], w=["qTz_o"])
            op("dve", lambda e, l=l, t=t: e.tensor_scalar(out=KT_sb[:, :, t * 128:(t + 1) * 128], in0=Bb[0][:, 512:1024].rearrange("p (c t) -> p c t", c=4),
                                                          scalar1=kg2[:, l:l + 1], scalar2=None, op0=ALU.mult), r=["B0", "kg2a", "kg2b"], w=["KT_%d" % t])
            QZ = ["qTz_e", "qTz_o", "qTz0"]
            if t % 2 == 1:
                op("dve", lambda e, c=c: e.tensor_reduce(out=kms[:], in_=KT_sb[:, :, c * 256:(c + 1) * 256], axis=AX.X, op=ALU.add),
                   r=["KT_%d" % (t - 1), "KT_%d" % t], w=["kms"])
                op("dve", lambda e, c=c: e.tensor_scalar(out=KM[0:64, :, c:c + 1], in0=kms[0:64, :].unsqueeze(2), scalar1=1.0 / 256, scalar2=None, op0=ALU.mult),
                   r=["kms", "KM"], w=["KM"])
                op("dve", lambda e, c=c: e.tensor_scalar(out=KM[64:128, :, 8 + c:9 + c], in0=kms[64:128, :].unsqueeze(2), scalar1=1.0 / 256, scalar2=None, op0=ALU.mult),
                   r=["kms", "KM"], w=["KM"])
            if D_:
                dump("d_qT", qTz[:, 0:4, :].rearrange("p c t -> p (c t)"), QZ)
                dump("d_kt", KT_sb[:, :, t * 128:(t + 1) * 128], ["KT_%d" % t])
            yield
            yield
            yield
            op("pool", lambda e: e.memset(selbp[:, :, 0:8], 0.0), w=["selbp"])
            op("pool", lambda e, t=t: e.tensor_copy(out=selbp[:, :, 8:12], in_=ctab[:, t, :, :]), r=["ctab"], w=["selbp_c"])
            if c >= 4:
                for p in range(4):
                    op("pe", lambda e, p=p: e.matmul(B[3][:, p * 16:(p + 1) * 16], lhsT=qTz[:, 2 * p, :], rhs=KM[:, p, :], start=(p == 0), stop=False, skip_group_check=True),
                       r=QZ + ["KM"], w=["B3"])
                    op("pe", lambda e, p=p: e.matmul(B[3][:, p * 16:(p + 1) * 16], lhsT=qTz[:, 2 * p + 1, :], rhs=KM[:, p, :], start=False, stop=True, skip_group_check=True),
                       r=QZ + ["KM"], w=["B3"])
                op("dve", lambda e: e.tensor_copy(out=g_sb[:], in_=B[3][:, 0:64]), r=["B3"], w=["g_sb"])
                g3 = g_sb[:].rearrange("p (h n) -> p h n", h=8)[:, :, 0:c]
                cmp4 = cmp[:, 0:8 * c * c].rearrange("p (h n m) -> p h n m", h=8, n=c)
                op("dve", lambda e, g3=g3, cmp4=cmp4, c=c: e.tensor_tensor(out=cmp4, in0=g3.unsqueeze(2).to_broadcast([128, 8, c, c]),
                                                                           in1=g3.unsqueeze(3).to_broadcast([128, 8, c, c]), op=ALU.is_gt), r=["g_sb"], w=["oa_a", "oa_b"])
                rk3 = rank[:].rearrange("p (h n) -> p h n", h=8)[:, :, 0:c]
                op("dve", lambda e, rk3=rk3, cmp4=cmp4: e.tensor_reduce(out=rk3, in_=cmp4, axis=AX.X, op=ALU.add), r=["oa_a", "oa_b"], w=["rank"])
                op("dve", lambda e, rk3=rk3, c=c: e.tensor_scalar(out=selbp[:, :, 0:c], in0=rk3, scalar1=2.5, scalar2=NEG, op0=ALU.is_gt, op1=ALU.mult),
                   r=["rank", "selbp"], w=["selbp"])
                if D_:
                    dump("d_g", g_sb[:], ["g_sb"])
            if D_:
                dump("d_selbp", selbp[:].rearrange("p h n -> p (h n)"), ["selbp", "selbp_c"])
            yield
            yield
            for hh in range(2):
                op("pe", lambda e, hh=hh: e.transpose(Bb[0][:, hh * 128:(hh + 1) * 128], selbp[:].rearrange("p h n -> p (h n)")[:, hh * 128:(hh + 1) * 128], ident[:]),
                   r=["selbp", "selbp_c", "ident"], w=["B0"])
            op("dve", lambda e: e.tensor_tensor(out=selbTz[:].rearrange("p (hh g) t -> p hh g t", g=4),
                                                in0=Bb[0][:, 0:256].rearrange("p (hh t) -> p hh t", hh=2).unsqueeze(2).to_broadcast([128, 2, 4, 128]),
                                                in1=gmask[:].unsqueeze(1).unsqueeze(3).to_broadcast([128, 2, 4, 128]), op=ALU.mult),
               r=["B0", "gmask"], w=["selbTz"])
            yield

        def moba(job):
            n, pi, sq, l, t = job["n"], job["pi"], job["sq"], job["l"], job["t"]
            nb_ = n % 2
            c = t // 2
            D_ = (dbg is not None and dbg == (sq, l, t))
            QZ = ["qTz_e", "qTz_o", "qTz0"]
            if t == 0 and n > 0:
                op("pool", lambda e: e.tensor_copy(out=V_sb[:, 0, :], in_=vst[:]), r=["vst"], w=["V_0"])
            groups = []
            for rnd in range(4):
                items = [(rnd, kt) for kt in range(t + 1)]
                for b0 in range(0, len(items), 2):
                    groups.append([rnd, items[b0:b0 + 2], False])
                groups[-1][2] = True
            bank_of = {}
            pv_next = [0]

            def qk(gi):
                rnd, grp, _ = groups[gi]
                pool_ = [4, 5] + ([3, 7] if job.get("a_done") else [])
                busy = set(bank_of[g2] for g2 in bank_of if g2 >= pv_next[0])
                bi = [b_ for b_ in pool_ if b_ not in busy][0]
                bank_of[gi] = bi
                bk = "B%d" % bi
                for si, (p, kt) in enumerate(grp):
                    sl = slice(si * 256, (si + 1) * 256)
                    op("pe", lambda e, bi=bi, sl=sl, p=p, kt=kt, si=si: e.matmul(B[bi][:, sl], lhsT=KT_sb[:, p, kt * 128:(kt + 1) * 128],
                                                                             rhs=qTz[:, 2 * p:2 * p + 2, :].rearrange("p a t -> p (a t)"),
                                                                             start=(si == 0), stop=False, skip_group_check=True),
                       r=["KT_%d" % kt] + QZ, w=[bk])
                    op("pe", lambda e, bi=bi, sl=sl, p=p, kt=kt, t=t: e.matmul(B[bi][:, sl], lhsT=Lk[:, kt, :], rhs=selbTz[:, 2 * p:2 * p + 2, :].rearrange("p a t -> p (a t)"),
                                                                           start=False, stop=(kt != t), skip_group_check=True),
                       r=["Lk", "selbTz"], w=[bk])
                    if kt == t:
                        op("pe", lambda e, bi=bi, sl=sl: e.matmul(B[bi][:, sl], lhsT=ident[:], rhs=cmask2[:], start=False, stop=True, skip_group_check=True),
                           r=["ident", "cmask2"], w=[bk])

            def ex(gi):
                rnd, grp, _ = groups[gi]
                bi = bank_of[gi]
                bk = "B%d" % bi
                pt = PT[gi % 2]
                ptk = "PT%d" % (gi % 2)
                n = len(grp) * 256
                op("act", lambda e, bi=bi, pt=pt, n=n: e.activation(out=pt[:, 0:n], in_=B[bi][:, 0:n], func=AF.Exp), r=[bk], w=[ptk])

            acc_of = {}

            def pv(gi):
                rnd, grp, _ = groups[gi]
                if rnd not in acc_of:
                    acc_of[rnd] = 1 if (rnd % 2 == 1 and job.get("h_done")) else 6
                ab = acc_of[rnd]
                abk = "B%d" % ab
                pt = PT[gi % 2]
                ptk = "PT%d" % (gi % 2)
                for si, (p, kt) in enumerate(grp):
                    sl = slice(si * 256, (si + 1) * 256)
                    op("pe", lambda e, kt=kt, p=p, pt=pt, sl=sl, t=t, ab=ab: e.matmul(B[ab][:, 0:256], lhsT=V_sb[:, kt, p * 128:(p + 1) * 128], rhs=pt[:, sl],
                                                                                  start=(kt == 0), stop=(kt == t), skip_group_check=True),
                       r=["V_%d" % kt, ptk], w=[abk])
                    op("pe", lambda e, kt=kt, pt=pt, sl=sl, t=t, ab=ab: e.matmul(B[ab][:, 256:512], lhsT=onesb[:], rhs=pt[:, sl],
                                                                             start=False, stop=(kt == t), skip_group_check=True),
                       r=["onesb", ptk], w=[abk])

            def norm_a(p):
                ab = acc_of[p]
                abk = "B%d" % ab
                op("act", lambda e, ab=ab: e.copy(out=rdn[0:64, 0, :], in_=B[ab][0:64, 256:384]), r=[abk], w=["rdn_a"])
                op("act", lambda e, ab=ab: e.copy(out=rdn[64:128, 0, :], in_=B[ab][64:128, 384:512]), r=[abk], w=["rdn_b"])
                op("act", lambda e, ab=ab: e.copy(out=oa[0:64, 0, :], in_=B[ab][0:64, 0:128]), r=[abk], w=["oa_a"])
                op("act", lambda e, ab=ab: e.copy(out=oa[64:128, 0, :], in_=B[ab][64:128, 128:256]), r=[abk], w=["oa_b"])

            def norm_b(p):
                op("dve", lambda e: e.reciprocal(out=rdn[:, 0, :], in_=rdn[:, 0, :]), r=["rdn_a", "rdn_b"], w=["rdn_a", "rdn_b"])
                op("dve", lambda e: e.tensor_tensor(out=oa[:, 0, :], in0=oa[:, 0, :], in1=rdn[:, 0, :], op=ALU.mult), r=["oa_a", "oa_b", "rdn_a", "rdn_b"], w=["oa_a", "oa_b"])
                op("dve", lambda e, p=p: e.tensor_tensor(out=mixT[:, 4 + p, :], in0=oa[:, 0, :], in1=sga[nb_][:, p * 128:(p + 1) * 128], op=ALU.mult),
                   r=["oa_a", "oa_b", "sga%d" % nb_], w=["mixT_a%d" % p])

            ng = len(groups)
            qk_next = [0]
            pending = [None]

            def fill():
                depth = 2 if job.get("a_done") else 1
                while qk_next[0] < ng and qk_next[0] <= pv_next[0] + depth:
                    qk(qk_next[0])
                    qk_next[0] += 1
                if qk_next[0] == ng:
                    job["qk_done"] = True
            fill()
            ex(0)
            for gi in range(ng):
                pv_next[0] = gi
                fill()
                if gi + 1 < ng:
                    ex(gi + 1)
                pv(gi)
                pv_next[0] = gi + 1
                if pending[0] is not None:
                    norm_b(pending[0])
                    pending[0] = None
                if groups[gi][2]:
                    norm_a(groups[gi][0])
                    pending[0] = groups[gi][0]
                yield
            if pending[0] is not None:
                norm_b(pending[0])
                pending[0] = None
            if D_:
                dump("d_mixT", mixT[:].rearrange("p c t -> p (c t)"), ["mixT_h", "mixT_a0", "mixT_a1", "mixT_a2", "mixT_a3"])
            yield


        def stageD(job):
            n, pi, sq, l, t = job["n"], job["pi"], job["sq"], job["l"], job["t"]
            xk = "x_%d" % t
            nxt = passes[pi + 1] if pi + 1 < len(passes) else None
            for hf in range(2):
                bi = 1 + hf
                for cc in range(8):
                    op("pe", lambda e, bi=bi, cc=cc, hf=hf: e.matmul(B[bi][:], lhsT=mixT[:, cc, :], rhs=wo_sb[:, cc, hf * 512:(hf + 1) * 512], start=(cc == 0), stop=(cc == 7)),
                       r=["mixT_h", "mixT_a0", "mixT_a1", "mixT_a2", "mixT_a3", "wo_%d" % cc], w=["B%d" % bi])
                op("dve", lambda e, bi=bi, hf=hf, t=t: e.tensor_tensor(out=x_sb[:, t, hf * 512:(hf + 1) * 512], in0=B[bi][:], in1=x_sb[:, t, hf * 512:(hf + 1) * 512], op=ALU.add),
                   r=["B%d" % bi, xk], w=[xk])
                yield
            if t == ntiles - 1 and nxt is not None and nxt[1] != l:
                load_w_out(nxt[1])
            if l == layers[-1]:
                out_dmas.append(dma("sp", lambda e, t=t, sq=sq: e.dma_start(out=y_d[sq, t * 128:(t + 1) * 128, :], in_=x_sb[:, t, :]), r=[xk], w=["y_%d_%d" % (sq, t)]))

        def run_all(g):
            for _ in g:
                pass

        run_all(stageA(jobs[0]))
        run_all(hgrn_pre(jobs[0]))
        run_all(moba_pre(jobs[0]))
        for n, job in enumerate(jobs):
            if n > 0 and job["t"] == 0:
                s.new_epoch()
            gens = {"H": hgrn(job), "M": moba(job)}
            has_next = n + 1 < len(jobs)
            if has_next:
                gens["A"] = stageA(jobs[n + 1])
            hp = mp = dstart = False
            if not has_next:
                job["a_done"] = True
            while gens:
                if "A" not in gens:
                    job["a_done"] = True
                if "H" not in gens:
                    job["h_done"] = True
                for key in list(gens):
                    try:
                        next(gens[key])
                    except StopIteration:
                        del gens[key]
                if not dstart and "H" not in gens and "M" not in gens:
                    gens["D"] = stageD(job)
                    dstart = True
                if has_next:
                    if not hp and "H" not in gens and "A" not in gens:
                        gens["HP"] = hgrn_pre(jobs[n + 1])
                        hp = True
                    if not mp and "A" not in gens and job.get("qk_done"):
                        gens["MP"] = moba_pre(jobs[n + 1])
                        mp = True
            if has_next:
                if not hp:
                    run_all(hgrn_pre(jobs[n + 1]))
                if not mp:
                    run_all(moba_pre(jobs[n + 1]))
            if not dstart:
                run_all(stageD(job))
        s.finish_wait("sp", out_dmas)
        s.emit(st)
    return nc


_PARAM_KEYS = ["norm_w", "w_in", "hg_lb_logits", "hg_norm_w", "q_norm_w", "k_norm_w", "w_out"]


def _launch(nc, x, params, consts, n_cores=8):
    in_maps = []
    for ci in range(n_cores):
        m = {"x": np.ascontiguousarray(x[ci * NSEQ:(ci + 1) * NSEQ])}
        m.update(params)
        m.update(consts)
        in_maps.append(m)
    res = run_bass_kernel_spmd(nc, in_maps, core_ids=list(range(n_cores)))
    return np.concatenate([r["y"] for r in res.results], axis=0)


FUSED = True


def kernel(x, norm_w, w_in, hg_lb_logits, hg_norm_w, q_norm_w, k_norm_w, w_out):
    x = np.ascontiguousarray(np.asarray(x, dtype=np.float32))
    params = dict(norm_w=norm_w, w_in=w_in, hg_lb_logits=hg_lb_logits, hg_norm_w=hg_norm_w,
                  q_norm_w=q_norm_w, k_norm_w=k_norm_w, w_out=w_out)
    params = {k: np.ascontiguousarray(np.asarray(v, dtype=np.float32)) for k, v in params.items()}
    consts = _consts()
    if FUSED:
        nc = _build([0, 1], 0)
        return _launch(nc, x, params, consts)
    y = x
    for l in range(2):
        nc = _build([l], 0)
        y = _launch(nc, y, params, consts)
    return y
```
